# Optimizing a Trainium2 kernel written in Bass

```python
import math
import jax
import jax.numpy as jnp
from jax import lax
import numpy as np

D_MODEL = 2048
BATCH = 2
SEQ = 8192
DEPTH = 1

N_HEADS = 16
N_KV_GROUPS = 2
HEADS_PER_GROUP = N_HEADS // N_KV_GROUPS
HEAD_DIM = 128
CMP_BLOCK = 32
CMP_STRIDE = 16
SEL_BLOCK = 64
SEL_TOP_N = 16
WINDOW = 512
Q_BLOCK = 128
N_OVERLAP = (SEL_BLOCK + CMP_BLOCK) // CMP_STRIDE - 1
FORCE_BONUS = 1.0e4
CONV_CH = D_MODEL // 2
CONV_WIDTH = 31
N_EXPERTS = 32
TOP_K = 4
D_FF = D_MODEL
SWIGLU_LIMIT = 7.0
SWIGLU_ALPHA = 1.702
EXPERT_BLOCK = 256
LN_EPS = 1e-5
DEEPNORM_ALPHA = (2 * DEPTH) ** 0.25
DEEPNORM_BETA = (8 * DEPTH) ** -0.25
NEG_INF = -1e30
TINY = 1e-30
Q_DIM = N_HEADS * HEAD_DIM
KV_DIM = N_KV_GROUPS * HEAD_DIM
NSA_GATE_DIM = N_HEADS * 3
GLU_DIM = 2 * CONV_CH
MERGE_DIM = 2 * D_MODEL
SPLITS = (Q_DIM, KV_DIM, KV_DIM, KV_DIM, KV_DIM, KV_DIM, KV_DIM, NSA_GATE_DIM, GLU_DIM, MERGE_DIM)
IN_DIM = sum(SPLITS)

kernel_name = "nsa_conformer_moe_hybrid_block"


def layer_norm(x, g, b):
    xf = x.astype(jnp.float32)
    mu = jnp.mean(xf, axis=-1, keepdims=True)
    var = jnp.mean(jnp.square(xf - mu), axis=-1, keepdims=True)
    return ((xf - mu) * lax.rsqrt(var + LN_EPS) * g + b).astype(x.dtype)


def masked_softmax(s, mask):
    s = jnp.where(mask, s.astype(jnp.float32), NEG_INF)
    m = jnp.max(s, axis=-1, keepdims=True)
    e = jnp.where(mask, jnp.exp(s - m), 0.0)
    return e / jnp.maximum(jnp.sum(e, axis=-1, keepdims=True), TINY)


def alibi_slopes():
    h = jnp.arange(1, N_HEADS + 1, dtype=jnp.float32)
    return jnp.exp2(-8.0 * h / N_HEADS).reshape(N_KV_GROUPS, HEADS_PER_GROUP)


def compress(kv, pe, w1, b1, w2, b2):
    B, S, G, dh = kv.shape
    n_cmp = (S - CMP_BLOCK) // CMP_STRIDE + 1
    idx = np.arange(n_cmp)[:, None] * CMP_STRIDE + np.arange(CMP_BLOCK)[None, :]
    blocks = kv[:, idx] + pe[None, None, :, None, :]
    flat = blocks.transpose(0, 1, 3, 2, 4).reshape(B, n_cmp, G, CMP_BLOCK * dh)
    h = jax.nn.gelu(flat @ w1 + b1)
    return h @ w2 + b2


def gather_blocks(kb, top):
    return jax.vmap(jax.vmap(lambda a, i: a[i]))(kb, top)


def nsa_attention(q, kc, vc, ks, vs, kw, vw, g_br):
    B, S, H, dh = q.shape
    n_chunk = S // Q_BLOCK
    n_cmp = kc.shape[1]
    n_selb = S // SEL_BLOCK
    n_top = min(SEL_TOP_N, n_selb)
    slopes = alibi_slopes()
    cmp_end = jnp.arange(n_cmp, dtype=jnp.int32) * CMP_STRIDE + (CMP_BLOCK - 1)
    lo = (np.arange(n_selb) * SEL_BLOCK - CMP_BLOCK) // CMP_STRIDE + 1
    ovl = lo[:, None] + np.arange(N_OVERLAP)[None, :]
    ovl = np.where((ovl >= 0) & (ovl < n_cmp), ovl, n_cmp)
    scale = 1.0 / math.sqrt(dh)
    qg = (q * scale).reshape(B, n_chunk, Q_BLOCK, N_KV_GROUPS, HEADS_PER_GROUP, dh).transpose(1, 0, 3, 4, 2, 5)
    gg = g_br.reshape(B, n_chunk, Q_BLOCK, N_KV_GROUPS, HEADS_PER_GROUP, 3).transpose(1, 0, 3, 4, 2, 5)
    ks_b = ks.reshape(B, n_selb, SEL_BLOCK, N_KV_GROUPS, dh).transpose(0, 3, 1, 2, 4)
    vs_b = vs.reshape(B, n_selb, SEL_BLOCK, N_KV_GROUPS, dh).transpose(0, 3, 1, 2, 4)
    kw_pad = jnp.pad(kw, ((0, 0), (WINDOW, 0), (0, 0), (0, 0)))
    vw_pad = jnp.pad(vw, ((0, 0), (WINDOW, 0), (0, 0), (0, 0)))
    blk = jnp.arange(n_selb, dtype=jnp.int32)

    def chunk(args):
        qb, gb, c = args
        t = c * Q_BLOCK + jnp.arange(Q_BLOCK, dtype=jnp.int32)
        dist = t[:, None] - cmp_end[None, :]
        s = jnp.einsum('bghqd,bngd->bghqn', qb, kc) - slopes[:, :, None, None] * dist
        p = masked_softmax(s, dist >= 0)
        o_cmp = jnp.einsum('bghqn,bngd->bghqd', p, vc)
        imp = jnp.pad(jnp.sum(p, axis=2), ((0, 0), (0, 0), (0, 0), (0, 1)))
        imp_sel = jnp.sum(imp[..., ovl], axis=-1)
        cur = t // SEL_BLOCK
        valid = blk[None, :] * SEL_BLOCK <= t[:, None]
        forced = (blk[None, :] == 0) | (blk[None, :] == cur[:, None]) | (blk[None, :] == cur[:, None] - 1)
        score = jnp.where(valid, imp_sel + jnp.where(forced, FORCE_BONUS, 0.0), -1.0)
        _, top = lax.top_k(score, n_top)
        k_g = gather_blocks(ks_b, top)
        v_g = gather_blocks(vs_b, top)
        pos = top[..., None] * SEL_BLOCK + jnp.arange(SEL_BLOCK, dtype=jnp.int32)
        d_s = t[None, None, :, None, None] - pos
        s2 = jnp.einsum('bghqd,bgqnkd->bghqnk', qb, k_g) - slopes[None, :, :, None, None, None] * d_s[:, :, None]
        p2 = masked_softmax(s2.reshape(B, N_KV_GROUPS, HEADS_PER_GROUP, Q_BLOCK, n_top * SEL_BLOCK),
                            (d_s >= 0)[:, :, None].reshape(B, N_KV_GROUPS, 1, Q_BLOCK, n_top * SEL_BLOCK))
        o_slc = jnp.einsum('bghqnk,bgqnkd->bghqd',
                           p2.reshape(B, N_KV_GROUPS, HEADS_PER_GROUP, Q_BLOCK, n_top, SEL_BLOCK), v_g)
        k_win = lax.dynamic_slice_in_dim(kw_pad, c * Q_BLOCK, Q_BLOCK + WINDOW, axis=1)
        v_win = lax.dynamic_slice_in_dim(vw_pad, c * Q_BLOCK, Q_BLOCK + WINDOW, axis=1)
        spos = c * Q_BLOCK - WINDOW + jnp.arange(Q_BLOCK + WINDOW, dtype=jnp.int32)
        dw = t[:, None] - spos[None, :]
        mask_w = (dw >= 0) & (dw < WINDOW) & (spos[None, :] >= 0)
        s3 = jnp.einsum('bghqd,bkgd->bghqk', qb, k_win) - slopes[:, :, None, None] * dw
        o_win = jnp.einsum('bghqk,bkgd->bghqd', masked_softmax(s3, mask_w), v_win)
        gs = jax.nn.sigmoid(gb.astype(jnp.float32))
        o = gs[..., 0:1] * o_cmp + gs[..., 1:2] * o_slc + gs[..., 2:3] * o_win
        return o.astype(q.dtype)

    out = lax.map(chunk, (qg, gg, jnp.arange(n_chunk, dtype=jnp.int32)))
    return out.transpose(1, 0, 4, 2, 3, 5).reshape(B, S, H * dh)


def conformer_conv(glu_in, conv_w, conv_b, ln_g, ln_b, w_proj):
    a, b = jnp.split(glu_in, 2, axis=-1)
    h = a * jax.nn.sigmoid(b)
    h = lax.conv_general_dilated(h, conv_w[:, None, :], window_strides=(1,),
                                 padding=[(CONV_WIDTH - 1, 0)],
                                 dimension_numbers=('NWC', 'WIO', 'NWC'),
                                 feature_group_count=CONV_CH) + conv_b
    h = jax.nn.silu(layer_norm(h, ln_g, ln_b))
    return h @ w_proj


def hybrid_mixer(x, w_in, cmp_pe, cmp_w1, cmp_b1, cmp_w2, cmp_b2, w_nsa_proj,
                 conv_w, conv_b, conv_ln_g, conv_ln_b, w_conv_proj, w_out):
    B, S, _ = x.shape
    proj = x @ w_in
    q, kc_r, vc_r, ks, vs, kw, vw, g_nsa, glu_in, g_merge = jnp.split(
        proj, np.cumsum(SPLITS)[:-1].tolist(), axis=-1)
    kvs = lambda t: t.reshape(B, S, N_KV_GROUPS, HEAD_DIM)
    kc = compress(kvs(kc_r), cmp_pe[0], cmp_w1[0], cmp_b1[0], cmp_w2[0], cmp_b2[0])
    vc = compress(kvs(vc_r), cmp_pe[1], cmp_w1[1], cmp_b1[1], cmp_w2[1], cmp_b2[1])
    o_nsa = nsa_attention(q.reshape(B, S, N_HEADS, HEAD_DIM), kc, vc, kvs(ks), kvs(vs),
                          kvs(kw), kvs(vw), g_nsa.reshape(B, S, N_HEADS, 3))
    y_a = o_nsa @ w_nsa_proj
    y_b = conformer_conv(glu_in, conv_w, conv_b, conv_ln_g, conv_ln_b, w_conv_proj)
    g_a, g_b = jnp.split(jax.nn.sigmoid(g_merge), 2, axis=-1)
    return (g_a * y_a + g_b * y_b) @ w_out


def moe(x, router_w, router_b, w_gate, b_gate, w_up, b_up, w_down, b_down):
    B, S, D = x.shape
    T = B * S
    xt = x.reshape(T, D)
    logits = (xt @ router_w + router_b).astype(jnp.float32)
    top_v, top_i = lax.top_k(logits, TOP_K)
    gate = jax.nn.softmax(top_v, axis=-1)
    A = T * TOP_K
    flat_e = top_i.reshape(A)
    flat_tok = jnp.arange(A, dtype=jnp.int32) // TOP_K
    flat_w = gate.reshape(A)
    order = jnp.argsort(flat_e)
    sorted_e = flat_e[order]
    counts = jnp.zeros((N_EXPERTS,), jnp.int32).at[flat_e].add(1)
    padded = (counts + EXPERT_BLOCK - 1) // EXPERT_BLOCK * EXPERT_BLOCK
    pad_end = jnp.cumsum(padded)
    pad_start = pad_end - padded
    start = jnp.cumsum(counts) - counts
    dest = pad_start[sorted_e] + (jnp.arange(A, dtype=jnp.int32) - start[sorted_e])
    P = -(-A // EXPERT_BLOCK) * EXPERT_BLOCK + N_EXPERTS * EXPERT_BLOCK
    n_blk = P // EXPERT_BLOCK
    buf_tok = jnp.full((P,), T, jnp.int32).at[dest].set(flat_tok[order])
    buf_w = jnp.zeros((P,), jnp.float32).at[dest].set(flat_w[order])
    blk_start = jnp.arange(n_blk, dtype=jnp.int32) * EXPERT_BLOCK
    blk_e = jnp.minimum(jnp.sum(blk_start[:, None] >= pad_end[None, :], axis=1), N_EXPERTS - 1)
    x_pad = jnp.concatenate([xt, jnp.zeros((1, D), xt.dtype)], axis=0)

    def expert_block(args):
        tok, e = args
        xb = x_pad[tok]
        g = jnp.minimum(xb @ w_gate[e] + b_gate[e], SWIGLU_LIMIT)
        u = jnp.clip(xb @ w_up[e] + b_up[e], -SWIGLU_LIMIT, SWIGLU_LIMIT)
        h = g * jax.nn.sigmoid(SWIGLU_ALPHA * g) * (u + 1.0)
        return h @ w_down[e] + b_down[e]

    out = lax.map(expert_block, (buf_tok.reshape(n_blk, EXPERT_BLOCK), blk_e)).reshape(P, D)
    out = (out * buf_w[:, None]).astype(x.dtype)
    y = jnp.zeros((T + 1, D), x.dtype).at[buf_tok].add(out)[:T]
    return y.reshape(B, S, D)


def setup_inputs(seed: int = 0) -> dict:
    key = jax.random.key(seed)
    k = jax.random.split(key, 32)
    L = DEPTH
    nrm = lambda kk, shape, sc: jax.random.normal(kk, shape, jnp.float32) * sc
    return {
        "x": nrm(k[0], (BATCH, SEQ, D_MODEL), 1.0),
        "w_in": nrm(k[1], (L, D_MODEL, IN_DIM), D_MODEL ** -0.5),
        "cmp_pe": nrm(k[2], (L, 2, CMP_BLOCK, HEAD_DIM), 0.1),
        "cmp_w1": nrm(k[3], (L, 2, CMP_BLOCK * HEAD_DIM, HEAD_DIM), (CMP_BLOCK * HEAD_DIM) ** -0.5),
        "cmp_b1": nrm(k[4], (L, 2, HEAD_DIM), 0.02),
        "cmp_w2": nrm(k[5], (L, 2, HEAD_DIM, HEAD_DIM), HEAD_DIM ** -0.5),
        "cmp_b2": nrm(k[6], (L, 2, HEAD_DIM), 0.02),
        "w_nsa_proj": nrm(k[7], (L, Q_DIM, D_MODEL), Q_DIM ** -0.5),
        "conv_w": nrm(k[8], (L, CONV_WIDTH, CONV_CH), CONV_WIDTH ** -0.5),
        "conv_b": nrm(k[9], (L, CONV_CH), 0.02),
        "conv_ln_g": 1.0 + nrm(k[10], (L, CONV_CH), 0.05),
        "conv_ln_b": nrm(k[11], (L, CONV_CH), 0.02),
        "w_conv_proj": nrm(k[12], (L, CONV_CH, D_MODEL), CONV_CH ** -0.5),
        "w_out": nrm(k[13], (L, D_MODEL, D_MODEL), DEEPNORM_BETA * D_MODEL ** -0.5),
        "ln1_g": 1.0 + nrm(k[14], (L, D_MODEL), 0.05),
        "ln1_b": nrm(k[15], (L, D_MODEL), 0.02),
        "router_w": nrm(k[16], (L, D_MODEL, N_EXPERTS), D_MODEL ** -0.5),
        "router_b": nrm(k[17], (L, N_EXPERTS), 0.01),
        "w_gate": nrm(k[18], (L, N_EXPERTS, D_MODEL, D_FF), D_MODEL ** -0.5),
        "b_gate": nrm(k[19], (L, N_EXPERTS, D_FF), 0.02),
        "w_up": nrm(k[20], (L, N_EXPERTS, D_MODEL, D_FF), D_MODEL ** -0.5),
        "b_up": nrm(k[21], (L, N_EXPERTS, D_FF), 0.02),
        "w_down": nrm(k[22], (L, N_EXPERTS, D_FF, D_MODEL), DEEPNORM_BETA * D_FF ** -0.5),
        "b_down": nrm(k[23], (L, N_EXPERTS, D_MODEL), 0.02),
        "ln2_g": 1.0 + nrm(k[24], (L, D_MODEL), 0.05),
        "ln2_b": nrm(k[25], (L, D_MODEL), 0.02),
    }


def reference(x, w_in, cmp_pe, cmp_w1, cmp_b1, cmp_w2, cmp_b2, w_nsa_proj,
              conv_w, conv_b, conv_ln_g, conv_ln_b, w_conv_proj, w_out, ln1_g, ln1_b,
              router_w, router_b, w_gate, b_gate, w_up, b_up, w_down, b_down, ln2_g, ln2_b):
    for l in range(DEPTH):
        m = hybrid_mixer(x, w_in[l], cmp_pe[l], cmp_w1[l], cmp_b1[l], cmp_w2[l], cmp_b2[l],
                         w_nsa_proj[l], conv_w[l], conv_b[l], conv_ln_g[l], conv_ln_b[l],
                         w_conv_proj[l], w_out[l])
        x = layer_norm(DEEPNORM_ALPHA * x + m, ln1_g[l], ln1_b[l])
        f = moe(x, router_w[l], router_b[l], w_gate[l], b_gate[l], w_up[l], b_up[l],
                w_down[l], b_down[l])
        x = layer_norm(DEEPNORM_ALPHA * x + f, ln2_g[l], ln2_b[l])
    return x
```

```python
import contextlib
import math
import numpy as np
import ml_dtypes
import concourse.bass as bass
import concourse.mybir as mybir
from concourse.bass_utils import run_bass_kernel_spmd

F32 = mybir.dt.float32
BF16 = mybir.dt.bfloat16
AF = mybir.ActivationFunctionType
ALU = mybir.AluOpType
AX = mybir.AxisListType

NCORES = 8
CPB = 4
G = 2
DH = 128
HALO = 30
BW = 128 + HALO
NEG = -30000.0
TINY = 1e-30

CFG_FULL = dict(S=8192, D=2048, H=16, TOPN=16, E=32, DFF=2048, CH=1024)


class Buf:
    __slots__ = ("name", "w", "r")

    def __init__(self, name=""):
        self.name = name
        self.w = {}
        self.r = {}


class V:
    __slots__ = ("ap", "b")

    def __init__(self, ap, b):
        self.ap = ap
        self.b = b

    def __getitem__(self, idx):
        return V(self.ap[idx], self.b)

    def re(self, pat, **kw):
        return V(self.ap.rearrange(pat, **kw), self.b)


class Prog:
    ENGS = ("pe", "act", "dve", "pool", "sp")

    def __init__(self, nc, n_ring=8):
        self.nc = nc
        self.es = contextlib.ExitStack()
        self.lists = {e: [] for e in self.ENGS}
        self.sems = {}
        self.cnt = {}
        for e in ("pe", "act", "dve", "pool"):
            self.sems[e] = self.es.enter_context(nc.semaphore("s_" + e))
            self.cnt[e] = 0
        self.ring = {}
        self.ring_pos = {}
        for q in ("sp", "pool"):
            self.ring[q] = []
            for i in range(n_ring):
                k = "d_%s%d" % (q, i)
                self.sems[k] = self.es.enter_context(nc.semaphore(k))
                self.cnt[k] = 0
                self.ring[q].append(k)
            self.ring_pos[q] = 0
        self.sems["cc"] = self.es.enter_context(nc.semaphore("s_cc"))
        self.cnt["cc"] = 0
        self.waited = {e: {} for e in self.ENGS}
        self.n_inst = 0

    def _need(self, eng, k, v, waits):
        if eng == "pe" and k == "pe":
            return
        if self.waited[eng].get(k, 0) >= v:
            return
        if waits.get(k, 0) < v:
            waits[k] = v

    def _deps(self, eng, reads, writes):
        waits = {}
        for b in reads:
            for k, v in b.w.items():
                self._need(eng, k, v, waits)
        for b in writes:
            for k, v in b.w.items():
                self._need(eng, k, v, waits)
            for k, v in b.r.items():
                self._need(eng, k, v, waits)
        for k, v in waits.items():
            self.waited[eng][k] = v
        return list(waits.items())

    def _commit(self, tok, reads, writes):
        k, v = tok
        for b in reads:
            if b.r.get(k, 0) < v:
                b.r[k] = v
        for b in writes:
            b.w[k] = v
            b.r = {}

    def op(self, eng, fn, reads=(), writes=()):
        waits = self._deps(eng, reads, writes)
        self.cnt[eng] += 1
        tok = (eng, self.cnt[eng])
        self.lists[eng].append(("op", fn, waits, eng, self.cnt[eng]))
        self._commit(tok, reads, writes)
        self.n_inst += 1

    def dma(self, q, fn, reads=(), writes=()):
        pos = self.ring_pos[q]
        self.ring_pos[q] = (pos + 1) % len(self.ring[q])
        k = self.ring[q][pos]
        waits = self._deps(q, reads, writes)
        prev = self.cnt[k]
        if prev > 0 and self.waited[q].get(k, 0) < prev:
            self.waited[q][k] = prev
            waits = waits + [(k, prev)]
        self.cnt[k] += 16
        tok = (k, self.cnt[k])
        self.lists[q].append(("dma", fn, waits, k, 16))
        self._commit(tok, reads, writes)
        self.n_inst += 1

    def collective(self, fn, reads=(), writes=()):
        waits = self._deps("pool", reads, writes)
        prev = self.cnt["cc"]
        if prev > 0 and self.waited["pool"].get("cc", 0) < prev:
            self.waited["pool"]["cc"] = prev
            waits = waits + [("cc", prev)]
        self.cnt["cc"] += 1
        tok = ("cc", self.cnt["cc"])
        self.lists["pool"].append(("dma", fn, waits, "cc", 1))
        self._commit(tok, reads, writes)

    def barrier(self):
        for eng in self.ENGS:
            waits = []
            for k, v in self.cnt.items():
                if v > 0 and self.waited[eng].get(k, 0) < v and not (eng == "pe" and k == "pe"):
                    self.waited[eng][k] = v
                    waits.append((k, v))
            self.lists[eng].append(("bar", None, waits, None, 0))

    def finish(self):
        nc = self.nc
        L = self.lists
        comp = ("pe", "act", "dve", "pool")
        W = {e: set() for e in comp}
        for eng in self.ENGS:
            for _, _, waits, _, _ in L[eng]:
                for k, v in waits:
                    if k in W:
                        W[k].add(v)
        rank = {e: {v: i + 1 for i, v in enumerate(sorted(W[e]))} for e in comp}
        self.n_incs = {e: len(W[e]) for e in comp}
        sems = self.sems

        def run(e, recs):
            for kind, fn, waits, key, n in recs:
                for k, v in waits:
                    e.wait_ge(sems[k], rank[k][v] if k in rank else v)
                if kind == "op":
                    r = fn(e)
                    if n in W[key]:
                        r.then_inc(sems[key], 1)
                elif kind == "dma":
                    fn(e).then_inc(sems[key], n)

        with nc.Block() as block:
            @block.tensor
            def _(e):
                run(e, L["pe"])

            @block.scalar
            def _(e):
                run(e, L["act"])

            @block.vector
            def _(e):
                run(e, L["dve"])

            @block.gpsimd
            def _(e):
                run(e, L["pool"])

            @block.sync
            def _(e):
                run(e, L["sp"])
        self.es.close()


class Arena:
    def __init__(self, P, name, ncols):
        self.t = P.es.enter_context(P.nc.sbuf_tensor(name, [128, ncols], F32))
        self.n = ncols
        self.pos = 0
        self.name = name

    def reset(self):
        self.pos = 0

    def alloc(self, shape, dtype, name=""):
        ne = int(np.prod(shape))
        n32 = ne if dtype == F32 else (ne + 1) // 2
        n32 = (n32 + 7) // 8 * 8
        assert self.pos + n32 <= self.n, "arena overflow: %s needs %d at %d/%d" % (name, n32, self.pos, self.n)
        ap = self.t[:, self.pos:self.pos + n32]
        self.pos += n32
        if dtype != F32:
            ap = ap.bitcast(dtype)
        ap = ap[:, 0:ne]
        if len(shape) == 2:
            ap = ap.rearrange("p (a b) -> p a b", b=shape[1])
        elif len(shape) == 3:
            ap = ap.rearrange("p (a b c) -> p a b c", b=shape[1], c=shape[2])
        return V(ap, Buf(name))


def derive(cfg):
    c = dict(cfg)
    S, D, H, E = c["S"], c["D"], c["H"], c["E"]
    c["NQ"] = S // 128
    c["NB"] = c["NQ"] // CPB
    c["NT"] = c["NB"] * 128
    c["HPG"] = H // G
    c["NCMP"] = (S - 32) // 16 + 1
    c["NSEL"] = S // 64
    c["DK"] = D // 128
    c["EPC"] = E // NCORES
    c["TALL"] = NCORES * c["NT"]
    off = {}
    o = 0
    for nm, w in (("q", H * DH), ("kc", G * DH), ("vc", G * DH), ("ks", G * DH), ("vs", G * DH),
                  ("kw", G * DH), ("vw", G * DH), ("gn", H * 3), ("glu", 2 * c["CH"]), ("mrg", 2 * D)):
        off[nm] = o
        o += w
    c["off"] = off
    c["IN_DIM"] = o
    return c


class _Stop(Exception):
    pass


def build_program(cfg, stop_after=None):
    try:
        return _build_program(cfg, stop_after)
    except _Stop as e:
        return e.args


def _build_program(cfg, stop_after=None):
    c = derive(cfg)
    S, D, H, E, DFF, CH, TOPN = c["S"], c["D"], c["H"], c["E"], c["DFF"], c["CH"], c["TOPN"]
    NQ, NB, NT, HPG, NCMP, NSEL, DK, EPC, TALL = (c["NQ"], c["NB"], c["NT"], c["HPG"], c["NCMP"],
                                                   c["NSEL"], c["DK"], c["EPC"], c["TALL"])
    off, IN_DIM = c["off"], c["IN_DIM"]
    CK = CH // 128
    FK = DFF // 128
    scale = 1.0 / math.sqrt(DH)
    ALPHA = 2.0 ** 0.25
    NTG = max(1, NB // 4)
    BPG = NB // NTG
    TG = BPG * 128
    assert TG <= 512

    nc = bass.Bass("TRN2", target_bir_lowering=False)
    P = Prog(nc)

    def din(name, shape, dt=F32):
        return V(nc.dram_tensor(name, list(shape), dt, kind="ExternalInput").ap(), Buf(name))

    def dtmp(name, shape, dt=F32):
        return V(nc.dram_tensor(name, list(shape), dt).ap(), Buf(name))

    xT_d = din("xT", [D, NB * BW])
    xtok_d = din("xtok", [NT, D])
    w_in_f = din("w_in", [D, IN_DIM])
    w_nsa_f = din("w_nsa", [H * DH, D])
    w_cp_f = din("w_cp", [CH, D])
    w_out_f = din("w_out", [D, D])
    peT_d = din("cmp_peT", [2, 128, 32])
    w1_d = din("cmp_w1", [2, 32 * 128, 128])
    b1_d = din("cmp_b1", [2, 128, 1])
    w2_d = din("cmp_w2", [2, 128, 128])
    b2_d = din("cmp_b2", [2, 128, 1])
    b2r_d = din("cmp_b2r", [2, 1, 128])
    convw_d = din("conv_wT", [CH, 31])
    convb_d = din("conv_b", [128, CH // 128])
    clng_d = din("conv_ln_g", [128, CH // 128])
    clnb_d = din("conv_ln_b", [128, CH // 128])
    ln1g_d = din("ln1_g", [1, D])
    ln1b_d = din("ln1_b", [1, D])
    ln2g_d = din("ln2_g", [1, D])
    ln2b_d = din("ln2_b", [1, D])
    rw_d = din("router_w", [D, E])
    rb_d = din("router_b", [1, E])
    wg_d = din("w_gate", [EPC, D, DFF])
    wu_d = din("w_up", [EPC, D, DFF])
    wd_d = din("w_down", [EPC, DFF, D])
    bg_d = din("b_gate", [EPC, 128, DFF // 128])
    bu_d = din("b_up", [EPC, 128, DFF // 128])
    bd_d = din("b_down", [EPC, 1, D])
    ident_d = din("ident", [128, 128])
    alibi_d = din("alibi_tab", [128, H * (NQ + 3)])
    wmask_d = din("wmask", [128, 8 * 128], BF16)
    camask_d = din("camask", [128, 4 * 128], BF16)
    MC = 32 * NB
    cmpb_d = din("cmpb", [128, H * MC])
    selF_d = din("selF", [128, 8 * NB])
    selV_d = din("selV", [128, 8 * NB])
    selm_d = din("selm", [1, EPC * E])
    out_d = V(nc.dram_tensor("out", [NT, D], F32, kind="ExternalOutput").ap(), Buf("out"))

    qT_s = dtmp("qT_s", [H * 128, NT], BF16)
    kT_send = dtmp("kT_send", [4 * G * 128, NT], BF16)
    kT_all = [dtmp("kT_all%d" % a, [CPB * 128, NT], BF16) for a in range(4 * G)]
    v_send = dtmp("v_send", [NT, 2 * G * 128], BF16)
    NVC = max(1, NT // 1024)
    NBV = NB // NVC
    v_all = [dtmp("v_all%d" % a, [CPB * NBV * 128, 2 * G * 128], BF16) for a in range(NVC)]
    onT_s = dtmp("onT_s", [H * 128, NT], BF16)
    x1_s = dtmp("x1_s", [NT, D])
    x1T_send = dtmp("x1T_send", [D, NT], BF16)
    x1T_all = [dtmp("x1T_all%d" % a, [NCORES * 128, NT], BF16) for a in range(DK)]
    gate_send = dtmp("gate_send", [NT, E])
    gate_all = dtmp("gate_all", [TALL, E])
    hT_s = dtmp("hT_s", [DFF, TALL], BF16)
    part_s = dtmp("part_s", [TALL, D])
    f_s = dtmp("f_s", [NT, D])

    AR = Arena(P, "arena", 39 * 1024)

    class _A:
        def __init__(self, dt):
            self.dt = dt

        def alloc(self, shape, name=""):
            return AR.alloc(shape, self.dt, name)

    A16 = _A(BF16)
    A32 = _A(F32)
    banks = [V(P.es.enter_context(nc.psum_tensor("pb%d" % i, [128, 512], F32))[:], Buf("pb%d" % i)) for i in range(8)]

    def ckpt(name):
        if stop_after == name:
            P.barrier()
            P.finish()
            raise _Stop(nc, c)

    def new_phase():
        P.barrier()
        AR.reset()

    def mm(out, lhsT, rhs, start, stop):
        P.op("pe", lambda e: e.matmul(out.ap, lhsT=lhsT.ap, rhs=rhs.ap, start=start, stop=stop),
             reads=[lhsT.b, rhs.b], writes=[out.b])

    def tr(out, in_, idn):
        P.op("pe", lambda e: e.transpose(out.ap, in_.ap, idn.ap), reads=[in_.b, idn.b], writes=[out.b])

    def act(out, in_, func, bias=None, scale=None, accum=None, eng="act"):
        kw = {}
        rd = [in_.b]
        wr = [out.b]
        if bias is not None:
            if isinstance(bias, V):
                kw["bias"] = bias.ap
                rd.append(bias.b)
            else:
                kw["bias"] = bias
        if scale is not None:
            if isinstance(scale, V):
                kw["scale"] = scale.ap
                rd.append(scale.b)
            else:
                kw["scale"] = scale
        if accum is not None:
            kw["accum_out"] = accum.ap
            wr.append(accum.b)
        P.op("act", lambda e: e.activation(out=out.ap, in_=in_.ap, func=func, **kw), reads=rd, writes=wr)

    def ts(out, in0, s1, s2, op0, op1=None, eng="dve"):
        rd = [in0.b]
        a1 = s1
        a2 = s2
        if isinstance(s1, V):
            a1 = s1.ap
            rd.append(s1.b)
        if isinstance(s2, V):
            a2 = s2.ap
            rd.append(s2.b)
        if op1 is None:
            P.op(eng, lambda e: e.tensor_scalar(out=out.ap, in0=in0.ap, scalar1=a1, scalar2=None, op0=op0),
                 reads=rd, writes=[out.b])
        else:
            P.op(eng, lambda e: e.tensor_scalar(out=out.ap, in0=in0.ap, scalar1=a1, scalar2=a2, op0=op0, op1=op1),
                 reads=rd, writes=[out.b])

    def stt(out, in0, s, in1, op0, op1):
        rd = [in0.b, in1.b]
        a = s
        if isinstance(s, V):
            a = s.ap
            rd.append(s.b)
        P.op("dve", lambda e: e.scalar_tensor_tensor(out=out.ap, in0=in0.ap, scalar=a, in1=in1.ap, op0=op0, op1=op1),
             reads=rd, writes=[out.b])

    def tt(out, in0, in1, op, eng="dve"):
        P.op(eng, lambda e: e.tensor_tensor(out=out.ap, in0=in0.ap, in1=in1.ap, op=op),
             reads=[in0.b, in1.b], writes=[out.b])

    def cp(out, in_, eng="dve"):
        P.op(eng, lambda e: e.tensor_copy(out=out.ap, in_=in_.ap), reads=[in_.b], writes=[out.b])

    def red(out, in_, op, negate=False):
        P.op("dve", lambda e: e.tensor_reduce(out=out.ap, in_=in_.ap, axis=AX.X, op=op, negate=negate),
             reads=[in_.b], writes=[out.b])

    def memset(out, val, eng="dve"):
        P.op(eng, lambda e: e.memset(out.ap, val), writes=[out.b])

    def ld(out, in_, q="sp"):
        P.dma(q, lambda e: e.dma_start(out=out.ap, in_=in_.ap), reads=[in_.b], writes=[out.b])

    def ldc(out, in_):
        P.dma("pool", lambda e: e.dma_start(out=out.ap, in_=in_.ap), reads=[in_.b], writes=[out.b])

    def ldb(out, in_, n):
        P.dma("sp", lambda e: e.dma_start(out=out.ap, in_=in_.ap.to_broadcast([128, n])), reads=[in_.b], writes=[out.b])

    grp4 = [[0, 1, 2, 3], [4, 5, 6, 7]]
    pairs = [[0, 4], [1, 5], [2, 6], [3, 7]]
    cc_n = [0]

    def ag8(src, dst):
        rows, cols = src.ap.shape
        cc_n[0] += 1
        mid = V(nc.dram_tensor("ccmid%d" % cc_n[0], [4 * rows, cols], src.ap.dtype).ap(), Buf("ccmid"))
        P.collective(lambda e: e.collective_compute("AllGather", ALU.bypass, replica_groups=grp4,
                                                    ins=[src.ap], outs=[mid.ap]), reads=[src.b], writes=[mid.b])
        P.collective(lambda e: e.collective_compute("AllGather", ALU.bypass, replica_groups=pairs,
                                                    ins=[mid.ap], outs=[dst.ap]), reads=[mid.b], writes=[dst.b])

    def rs8(src, dst):
        rows, cols = src.ap.shape
        cc_n[0] += 1
        mid = V(nc.dram_tensor("ccmid%d" % cc_n[0], [rows // 2, cols], src.ap.dtype).ap(), Buf("ccmid"))
        P.collective(lambda e: e.collective_compute("ReduceScatter", ALU.add, replica_groups=pairs,
                                                    ins=[src.ap], outs=[mid.ap]), reads=[src.b], writes=[mid.b])
        P.collective(lambda e: e.collective_compute("ReduceScatter", ALU.add, replica_groups=grp4,
                                                    ins=[mid.ap], outs=[dst.ap]), reads=[mid.b], writes=[dst.b])


    ckpt('p0')
    def const_tile(name, shape, dt):
        return V(P.es.enter_context(nc.sbuf_tensor(name, list(shape), dt))[:], Buf(name))

    ident = const_tile("ident_sb", [128, 128], F32)
    ld(ident, ident_d)
    identb = const_tile("identb_sb", [128, 128], BF16)
    cp(identb, ident)
    gsig = const_tile("gsig", [128, NB, H * 3], F32)
    ones_f = const_tile("ones_f", [128, 128], F32)
    memset(ones_f, 1.0)
    eps_t = const_tile("eps_t", [128, 1], F32)
    memset(eps_t, 1e-5)

    w_in_v = w_in_f.re("(k p) n -> p k n", p=128)

    new_phase()
    xT_v = xT_d.re("(k p) n -> p k n", p=128)
    xg = [A16.alloc([DK, BPG * BW], "xg%d" % i) for i in range(2)]
    wt = [A16.alloc([DK, 512], "wt%d" % i) for i in range(2)]
    stg = [A16.alloc([512], "stg%d" % i) for i in range(2)]
    stg32 = A32.alloc([512], "stg32")
    wcount = [0]

    def load_w_cols(c0, n):
        t = wt[wcount[0] % 2]
        wcount[0] += 1
        ldc(t[:, :, 0:n], w_in_v[:, :, c0:c0 + n])
        return t

    kT_send_v = kT_send.re("(a p) n -> p a n", p=128)
    qT_v = qT_s.re("(a p) n -> p a n", p=128)
    ktypes = ("kc", "vc", "ks", "kw")
    for tg in range(NTG):
        x = xg[tg % 2]
        ldc(x, xT_v[:, :, tg * BPG * BW:(tg + 1) * BPG * BW])
        x3 = x.re("p k (b w) -> p k b w", w=BW)
        fm_cols = [(off["q"] + h * 128, ("q", h)) for h in range(H)]
        for ti, nm in enumerate(ktypes):
            for g in range(G):
                fm_cols.append((off[nm] + g * 128, ("k", ti * G + g)))
        for ci in range(0, len(fm_cols), 4):
            chunk = fm_cols[ci:ci + 4]
            contiguous = all(chunk[j][0] == chunk[0][0] + 128 * j for j in range(len(chunk)))
            for j, (c0, dest) in enumerate(chunk):
                if contiguous:
                    if j == 0:
                        w = load_w_cols(chunk[0][0], 128 * len(chunk))
                    wsl = w[:, :, j * 128:(j + 1) * 128]
                else:
                    w = load_w_cols(c0, 128)
                    wsl = w[:, :, 0:128]
                pb = banks[(ci + j) % 2]
                po = pb[:, 0:TG].re("p (b w) -> p b w", w=128)
                for k in range(DK):
                    mm(po, wsl[:, k, :], x3[:, k, :, HALO:BW], k == 0, k == DK - 1)
                st = stg[(ci + j) % 2]
                cp(st[:, 0:TG], pb[:, 0:TG], eng="dve" if j % 2 == 0 else "dve")
                if dest[0] == "q":
                    ld(qT_v[:, dest[1], tg * TG:(tg + 1) * TG], st[:, 0:TG])
                else:
                    ld(kT_send_v[:, dest[1], tg * TG:(tg + 1) * TG], st[:, 0:TG])
        wv = []
        for nm in ("vs", "vw"):
            wv.append(load_w_cols(off[nm], G * 128))
            for b in range(BPG):
                pb = banks[2 + b % 2]
                for k in range(DK):
                    mm(pb[:, 0:G * 128], x3[:, k, b, HALO:BW], wv[-1][:, k, 0:G * 128], k == 0, k == DK - 1)
                st = stg[b % 2]
                cp(st[:, 0:G * 128], pb[:, 0:G * 128])
                tsel = 0 if nm == "vs" else 1
                row0 = (tg * BPG + b) * 128
                ld(v_send[row0:row0 + 128, tsel * G * 128:(tsel + 1) * G * 128], st[:, 0:G * 128])
        wgn = load_w_cols(off["gn"], H * 3)
        for b in range(BPG):
            pb = banks[2 + b % 2]
            for k in range(DK):
                mm(pb[:, 0:H * 3], x3[:, k, b, HALO:BW], wgn[:, k, 0:H * 3], k == 0, k == DK - 1)
            act(gsig[:, tg * BPG + b, :], pb[:, 0:H * 3], AF.Sigmoid)

    ckpt('pA0')
    for a_ in range(4 * G):
        P.collective(lambda e, a_=a_: e.collective_compute("AllGather", ALU.bypass, replica_groups=grp4,
                                                           ins=[kT_send.ap[a_ * 128:(a_ + 1) * 128, :]],
                                                           outs=[kT_all[a_].ap]),
                     reads=[kT_send.b], writes=[kT_all[a_].b])
    for a_ in range(NVC):
        P.collective(lambda e, a_=a_: e.collective_compute("AllGather", ALU.bypass, replica_groups=grp4,
                                                           ins=[v_send.ap[a_ * NBV * 128:(a_ + 1) * NBV * 128, :]],
                                                           outs=[v_all[a_].ap]),
                     reads=[v_send.b], writes=[v_all[a_].b])

    ckpt('pA')
    kT_all_v = [t_.re("(r p) n -> p r n", p=128) for t_ in kT_all]
    v_all_v = [t_.re("(r i p) c -> p r i c", p=128, i=NBV) for t_ in v_all]
    onT_v = onT_s.re("(a p) n -> p a n", p=128)
    NCH = (NCMP + 127) // 128
    HB = min(4, HPG)
    KcT = const_tile("KcT", [128, 512], BF16)
    Vc = const_tile("Vc", [128, NCH, 130], BF16)

    for g in range(G):
        new_phase()
        Xc = A16.alloc([S], "Xc")
        w1s = A16.alloc([32, 128], "w1s")
        w2s = A16.alloc([128], "w2s")
        peT = A16.alloc([32], "peT")
        hid = A16.alloc([512], "hid")
        b1s = A32.alloc([1], "b1s")
        b2s = A32.alloc([1], "b2s")
        b2r = A32.alloc([128], "b2r")
        pbias = A32.alloc([1], "pbias")
        gx = A32.alloc([512], "gx")
        gy = A32.alloc([512], "gy")
        ckpt('c00')
        cp(Vc[:, :, 128:130], ones_f[:, 0:2 * NCH].re("p (a b) -> p a b", b=2))
        ckpt('c0')
        for t in range(2):
            d4 = Xc.re("p (i r q) -> p i r q", r=CPB, q=128)
            for r in range(CPB):
                ld(d4[:, :, r, :], kT_all_v[t * G + g][:, r, :].re("p (i q) -> p i q", q=128))
            ckpt('c0a')
            ldc(w1s, w1_d[t].re("(l p) o -> p l o", p=128))
            ldc(w2s, w2_d[t])
            ldc(peT, peT_d[t])
            ckpt('c0b')
            ld(b1s, b1_d[t])
            ld(b2s, b2_d[t])
            ldb(b2r, b2r_d[t], 128)
            ckpt('c1')
            ph = banks[0]
            pp = banks[1]
            for l in range(32):
                mm(ph[:, 0:NCMP], w1s[:, l, :], Xc[:, l:l + 16 * (NCMP - 1) + 1:16], l == 0, l == 31)
            ckpt('c2')
            for l in range(32):
                mm(pp[:, 0:1], w1s[:, l, :], peT[:, l:l + 1], l == 0, l == 31)
            ckpt('c3')
            tt(pbias, pp[:, 0:1], b1s, ALU.add)
            act(gx[:, 0:NCMP], ph[:, 0:NCMP], AF.Identity, bias=pbias)
            tt(gy[:, 0:NCMP], gx[:, 0:NCMP], gx[:, 0:NCMP], ALU.mult)
            ts(gy[:, 0:NCMP], gy[:, 0:NCMP], 0.044715, 1.0, ALU.mult, ALU.add)
            tt(gy[:, 0:NCMP], gy[:, 0:NCMP], gx[:, 0:NCMP], ALU.mult)
            act(gy[:, 0:NCMP], gy[:, 0:NCMP], AF.Tanh, scale=0.7978845608028654)
            stt(gy[:, 0:NCMP], gy[:, 0:NCMP], 1.0, gx[:, 0:NCMP], ALU.add, ALU.mult)
            ts(hid[:, 0:NCMP], gy[:, 0:NCMP], 0.5, None, ALU.mult)
            ckpt('c4')
            if t == 0:
                mm(pp[:, 0:NCMP], w2s, hid[:, 0:NCMP], True, True)
                act(KcT[:, 0:NCMP], pp[:, 0:NCMP], AF.Identity, bias=b2s)
            else:
                for ch in range(NCH):
                    n0 = ch * 128
                    nn = min(128, NCMP - n0)
                    mm(pp[0:nn, 0:128], hid[:, n0:n0 + nn], w2s, True, True)
                    tt(Vc[0:nn, ch, 0:128], pp[0:nn, 0:128], b2r[0:nn, :], ALU.add)
        ckpt('pCc')
        new_phase()
        KsT = A16.alloc([S], "KsT")
        KwT = A16.alloc([S], "KwT")
        Vs = A16.alloc([NQ, 130], "Vs")
        Vw = A16.alloc([NQ, 130], "Vw")
        QTb = [A16.alloc([HPG, 128], "QT%d" % i_) for i_ in range(2)]
        wmask = A16.alloc([8, 128], "wmask")
        camask = A16.alloc([4, 128], "camask")
        alibi = A32.alloc([H, NQ + 3], "alibi")
        cmpb = A32.alloc([HPG, MC], "cmpb")
        selF = A32.alloc([8 * NB], "selF")
        selV = A32.alloc([8 * NB], "selV")
        ld(wmask, wmask_d.re("p (a b) -> p a b", b=128))
        ld(camask, camask_d.re("p (a b) -> p a b", b=128))
        ld(alibi, alibi_d.re("p (a b) -> p a b", b=NQ + 3))
        ld(cmpb, cmpb_d.re("p (a b) -> p a b", b=MC)[:, g * HPG:(g + 1) * HPG, :])
        ld(selF, selF_d)
        ld(selV, selV_d)
        for (dst, ti) in ((KsT, 2), (KwT, 3)):
            d4 = dst.re("p (i r q) -> p i r q", r=CPB, q=128)
            for r in range(CPB):
                ld(d4[:, :, r, :], kT_all_v[ti * G + g][:, r, :].re("p (i q) -> p i q", q=128))
        for (dst, tsel) in ((Vs, 0), (Vw, 1)):
            cp(dst[:, :, 128:130], ones_f[:, 0:2 * NQ].re("p (a b) -> p a b", b=2))
            d4 = dst.re("p (i r) c -> p i r c", r=CPB)
            for r in range(CPB):
                for a_ in range(NVC):
                    ld(d4[:, a_ * NBV:(a_ + 1) * NBV, r, 0:128],
                       v_all_v[a_][:, r, :, (tsel * G + g) * 128:(tsel * G + g + 1) * 128])
        sc = A32.alloc([512], "sc")
        ex = A32.alloc([512], "ex")
        imp = A32.alloc([4 * 8 * NB + 8], "imp")
        isel = A32.alloc([8 * NB], "isel")
        score = A32.alloc([8 * NB], "score")
        scr2 = A32.alloc([8 * NB], "scr2")
        mx8 = A32.alloc([8], "mx8")
        msk = A32.alloc([8 * NB], "msk")
        mE = [A32.alloc([2, 64], "mE%d" % i_) for i_ in range(2)]
        MT = A16.alloc([NQ, 128], "MT")
        eT = A16.alloc([NCH, 128], "eT")
        pT = [A16.alloc([HB, 128], "pT%d" % i) for i in range(2)]
        small = A32.alloc([16], "small")
        ocmp = A32.alloc([HPG, 129], "ocmp")
        oacc = A32.alloc([128], "oacc")
        wcol = A32.alloc([8], "wcol")
        onT_st = A16.alloc([HPG, 128], "onT_st")
        for i in range(NB):
            Ni = min(32 * i + 31, NCMP)
            Ji = min(8 * i + 8, NSEL)
            m0 = 32 * (NB - 1 - i)
            f0 = 8 * (NB - 1 - i)
            nch_i = (Ni + 127) // 128
            memset(imp, 0.0)
            QT = QTb[i % 2]
            ld(QT, qT_v[:, g * HPG:(g + 1) * HPG, i * 128:(i + 1) * 128])
            ckpt('a0')
            for h in range(HPG):
                ps = banks[h % 2]
                mm(ps[:, 0:Ni], QT[:, h, :], KcT[:, 0:Ni], True, True)
                stt(sc[:, 0:Ni], ps[:, 0:Ni], scale, cmpb[:, h, m0:m0 + Ni], ALU.mult, ALU.add)
                red(small[:, 0:1], sc[:, 0:Ni], ALU.max)
                ts(small[:, 1:2], small[:, 0:1], -1.0, 20000.0, ALU.mult, ALU.min)
                act(ex[:, 0:Ni], sc[:, 0:Ni], AF.Exp, bias=small[:, 1:2])
                red(small[:, 2:3], ex[:, 0:Ni], ALU.add)
                ts(small[:, 3:4], small[:, 2:3], TINY, None, ALU.max)
                P.op("dve", lambda e, small=small: e.reciprocal(out=small.ap[:, 4:5], in_=small.ap[:, 3:4]),
                     reads=[small.b], writes=[small.b])
                stt(imp[:, 1:1 + Ni], ex[:, 0:Ni], small[:, 4:5], imp[:, 1:1 + Ni], ALU.mult, ALU.add)
                ckpt('a0c')
                for ch in range(nch_i):
                    n0 = ch * 128
                    nn = min(128, Ni - n0)
                    pt = banks[2 + ch % 2]
                    tr(pt[0:nn, 0:128], ex[:, n0:n0 + nn], ident)
                    cp(eT[0:nn, ch, :], pt[0:nn, 0:128])
                ckpt('a0d')
                po = banks[4]
                for ch in range(nch_i):
                    n0 = ch * 128
                    nn = min(128, Ni - n0)
                    mm(po[:, 0:129], eT[0:nn, ch, :], Vc[0:nn, ch, 0:129], ch == 0, ch == nch_i - 1)
                cp(ocmp[:, h, :], po[:, 0:129])
            ckpt('a1')
            impv = imp[:, 0:4 * Ji].re("p (j f) -> p j f", f=4)
            red(isel[:, 0:Ji], impv, ALU.add)
            tt(isel[:, 0:Ji], isel[:, 0:Ji], imp[:, 4:4 + 4 * Ji].re("p (j f) -> p j f", f=4)[:, :, 0], ALU.add)
            tt(score[:, 0:Ji], isel[:, 0:Ji], selF[:, f0:f0 + Ji], ALU.add)
            ts(score[:, 0:1], score[:, 0:1], 1.0e4, None, ALU.add)
            if Ji > TOPN:
                cur = score
                for rnd in range(TOPN // 8):
                    P.op("dve", lambda e, cur=cur, Ji=Ji, mx8=mx8: e.max(out=mx8.ap, in_=cur.ap[:, 0:Ji]), reads=[cur.b], writes=[mx8.b])
                    if rnd < TOPN // 8 - 1:
                        P.op("dve", lambda e, cur=cur, Ji=Ji, mx8=mx8, scr2=scr2: e.match_replace(out=scr2.ap[:, 0:Ji], in_to_replace=mx8.ap,
                                                                      in_values=cur.ap[:, 0:Ji], imm_value=-1.0e9),
                             reads=[cur.b, mx8.b], writes=[scr2.b])
                        cur = scr2
                stt(msk[:, 0:Ji], score[:, 0:Ji], mx8[:, 7:8], selV[:, f0:f0 + Ji], ALU.is_ge, ALU.mult)
            else:
                cp(msk[:, 0:Ji], selV[:, f0:f0 + Ji])
            ckpt('a1b')
            ntile = min(4 * i + 4, NQ)
            for j2 in range(ntile):
                pm = banks[2 + j2 % 2]
                me = mE[j2 % 2]
                P.op("dve", lambda e, me=me, j2=j2, msk=msk: e.tensor_copy(
                    out=me.ap, in_=msk.ap[:, 2 * j2:2 * j2 + 2].unsqueeze(2).to_broadcast([128, 2, 64])),
                    reads=[msk.b], writes=[me.b])
                tr(pm[:, 0:128], me.re("p a b -> p (a b)"), ident)
                if j2 >= 4 * i:
                    tt(MT[:, j2, :], pm[:, 0:128], camask[:, j2 - 4 * i, :], ALU.mult)
                else:
                    cp(MT[:, j2, :], pm[:, 0:128])
            ckpt('a2')
            for hb in range(HPG // HB):
                for br in range(2):
                    if br == 0:
                        tiles = list(range(ntile))
                        KT, VV = KsT, Vs
                    else:
                        tiles = [j for j in range(4 * i - 4, 4 * i + 4) if 0 <= j < NQ]
                        KT, VV = KwT, Vw
                    pos_ = [banks[4 + hh] for hh in range(HB)]
                    for n_, j2 in enumerate(tiles):
                        pS = banks[n_ % 2]
                        u = 4 * i - j2 + 3
                        mm(pS[:, 0:HB * 128].re("p (h q) -> p h q", q=128), KT[:, j2 * 128:(j2 + 1) * 128],
                           QT[:, hb * HB:(hb + 1) * HB, :], True, True)
                        pt_ = pT[n_ % 2]
                        for hh in range(HB):
                            hglob = g * HPG + hb * HB + hh
                            act(pt_[:, hh, :], pS[:, hh * 128:(hh + 1) * 128], AF.Exp,
                                bias=alibi[:, hglob, u:u + 1], scale=scale)
                        if br == 0:
                            mk = MT[:, j2, :]
                        else:
                            mk = wmask[:, j2 - (4 * i - 4), :]
                        for hh in range(HB):
                            tt(pt_[:, hh, :], pt_[:, hh, :], mk, ALU.mult, eng="pool" if hh % 2 else "dve")
                        for hh in range(HB):
                            mm(pos_[hh][:, 0:129], pt_[:, hh, :], VV[:, j2, 0:129], n_ == 0, n_ == len(tiles) - 1)
                    for hh in range(HB):
                        h = hb * HB + hh
                        hglob = g * HPG + h
                        po = pos_[hh]
                        ts(wcol[:, 0:1], po[:, 128:129], TINY, None, ALU.max)
                        P.op("dve", lambda e, wcol=wcol: e.reciprocal(out=wcol.ap[:, 1:2], in_=wcol.ap[:, 0:1]),
                             reads=[wcol.b], writes=[wcol.b])
                        tt(wcol[:, 2:3], wcol[:, 1:2], gsig[:, i, hglob * 3 + 1 + br:hglob * 3 + 2 + br], ALU.mult)
                        if br == 0:
                            ts(wcol[:, 3:4], ocmp[:, h, 128:129], TINY, None, ALU.max)
                            P.op("dve", lambda e, wcol=wcol: e.reciprocal(out=wcol.ap[:, 4:5], in_=wcol.ap[:, 3:4]),
                                 reads=[wcol.b], writes=[wcol.b])
                            tt(wcol[:, 5:6], wcol[:, 4:5], gsig[:, i, hglob * 3:hglob * 3 + 1], ALU.mult)
                            ts(ocmp[:, h, 0:128], ocmp[:, h, 0:128], wcol[:, 5:6], None, ALU.mult)
                            stt(ocmp[:, h, 0:128], po[:, 0:128], wcol[:, 2:3], ocmp[:, h, 0:128], ALU.mult, ALU.add)
                        else:
                            stt(oacc, po[:, 0:128], wcol[:, 2:3], ocmp[:, h, 0:128], ALU.mult, ALU.add)
                            ptr = banks[2 + hh % 2]
                            tr(ptr[:, 0:128], oacc, ident)
                            cp(onT_st[:, h, :], ptr[:, 0:128])
            ckpt('a3')
            ld(onT_v[:, g * HPG:(g + 1) * HPG, i * 128:(i + 1) * 128], onT_st)

    ckpt('pC')
    w_nsa_v = w_nsa_f.re("(k p) n -> p k n", p=128)
    w_cp_v = w_cp_f.re("(k p) n -> p k n", p=128)
    w_out_v = w_out_f.re("(k p) n -> p k n", p=128)
    x1T_send_v = x1T_send.re("(k p) n -> p k n", p=128)
    convw_v = convw_d.re("(k p) t -> p k t", p=128)
    for tg in range(NTG):
        new_phase()
        NW = BPG * BW
        x = A16.alloc([DK, NW], "xgD")
        ldc(x, xT_v[:, :, tg * NW:(tg + 1) * NW])
        x3 = x.re("p k (b w) -> p k b w", w=BW)
        wtD = [A16.alloc([DK, 128], "wtD%d" % i_) for i_ in range(2)]
        mT = A16.alloc([DK, TG], "mT")
        mark = AR.pos
        convw = A32.alloc([CK, 31], "convw")
        convb = A32.alloc([CK], "convb")
        clng = A32.alloc([CK], "clng")
        clnb = A32.alloc([CK], "clnb")
        ld(convw, convw_v)
        ld(convb, convb_d)
        ld(clng, clng_d)
        ld(clnb, clnb_d)
        hglu = A32.alloc([NW], "hglu")
        sg = A32.alloc([NW], "sg")
        hc = A32.alloc([CK, TG], "hc")
        hsq = A32.alloc([TG], "hsq")
        mean = A32.alloc([TG], "mean")
        rstd = A32.alloc([TG], "rstd")
        sT = A16.alloc([CK, TG], "sT")
        p_sum = banks[6]
        p_sq = banks[7]
        for ck in range(CK):
            wa = wtD[0]
            wb = wtD[1]
            ldc(wa[:, :, 0:128], w_in_v[:, :, off["glu"] + ck * 128:off["glu"] + (ck + 1) * 128])
            ldc(wb[:, :, 0:128], w_in_v[:, :, off["glu"] + CH + ck * 128:off["glu"] + CH + (ck + 1) * 128])
            for b0 in range(0, BPG, 3):
                nb_ = min(3, BPG - b0)
                pa = banks[0]
                pbk = banks[1]
                oa = pa[:, 0:nb_ * BW].re("p (b w) -> p b w", w=BW)
                ob = pbk[:, 0:nb_ * BW].re("p (b w) -> p b w", w=BW)
                for k in range(DK):
                    mm(oa, wa[:, k, 0:128], x3[:, k, b0:b0 + nb_, :], k == 0, k == DK - 1)
                for k in range(DK):
                    mm(ob, wb[:, k, 0:128], x3[:, k, b0:b0 + nb_, :], k == 0, k == DK - 1)
                act(sg[:, b0 * BW:(b0 + nb_) * BW], pbk[:, 0:nb_ * BW], AF.Sigmoid)
                tt(hglu[:, b0 * BW:(b0 + nb_) * BW], pa[:, 0:nb_ * BW], sg[:, b0 * BW:(b0 + nb_) * BW], ALU.mult)
            h3 = hglu.re("p (b w) -> p b w", w=BW)
            hcv = hc[:, ck, :].re("p (b q) -> p b q", q=128)
            ts(hcv, h3[:, :, 0:128], convw[:, ck, 0:1], convb[:, ck:ck + 1], ALU.mult, ALU.add)
            for tap in range(1, 31):
                stt(hcv, h3[:, :, tap:tap + 128], convw[:, ck, tap:tap + 1], hcv, ALU.mult, ALU.add)
            act(hsq, hc[:, ck, :], AF.Square)
            mm(p_sum[:, 0:TG], ones_f, hc[:, ck, :], ck == 0, ck == CK - 1)
            mm(p_sq[:, 0:TG], ones_f, hsq, ck == 0, ck == CK - 1)
        ckpt('d1')
        ts(mean, p_sum[:, 0:TG], 1.0 / CH, None, ALU.mult)
        tt(rstd, mean, mean, ALU.mult)
        stt(rstd, p_sq[:, 0:TG], 1.0 / CH, rstd, ALU.mult, ALU.subtract)
        ts(rstd, rstd, 0.0, None, ALU.max)
        act(rstd, rstd, AF.Sqrt, bias=eps_t)
        P.op("dve", lambda e, rstd=rstd: e.reciprocal(out=rstd.ap, in_=rstd.ap), reads=[rstd.b], writes=[rstd.b])
        for ck in range(CK):
            tt(hc[:, ck, :], hc[:, ck, :], mean, ALU.subtract)
            tt(hc[:, ck, :], hc[:, ck, :], rstd, ALU.mult)
            act(sT[:, ck, :], hc[:, ck, :], AF.Silu, bias=clnb[:, ck:ck + 1], scale=clng[:, ck:ck + 1])
        ckpt('d2')
        onT = A16.alloc([H, TG], "onT")
        ld(onT, onT_v[:, :, tg * TG:(tg + 1) * TG])
        wn = [A16.alloc([H, 128], "wn%d" % i_) for i_ in range(2)]
        wc = [A16.alloc([CK, 128], "wc%d" % i_) for i_ in range(2)]
        ga = A32.alloc([TG], "ga")
        gb = A32.alloc([TG], "gb")
        for dm in range(DK):
            wa = wtD[0]
            wb = wtD[1]
            ldc(wa[:, :, 0:128], w_in_v[:, :, off["mrg"] + dm * 128:off["mrg"] + (dm + 1) * 128])
            ldc(wb[:, :, 0:128], w_in_v[:, :, off["mrg"] + D + dm * 128:off["mrg"] + D + (dm + 1) * 128])
            wnn = wn[dm % 2]
            wcc = wc[dm % 2]
            ldc(wnn, w_nsa_v[:, :, dm * 128:(dm + 1) * 128])
            ldc(wcc, w_cp_v[:, :, dm * 128:(dm + 1) * 128])
            pga, pgb, pya, pyb = banks[0], banks[1], banks[2], banks[3]
            oga = pga[:, 0:TG].re("p (b q) -> p b q", q=128)
            ogb = pgb[:, 0:TG].re("p (b q) -> p b q", q=128)
            for k in range(DK):
                mm(oga, wa[:, k, 0:128], x3[:, k, :, HALO:BW], k == 0, k == DK - 1)
            for k in range(DK):
                mm(ogb, wb[:, k, 0:128], x3[:, k, :, HALO:BW], k == 0, k == DK - 1)
            for k in range(H):
                mm(pya[:, 0:TG], wnn[:, k, :], onT[:, k, :], k == 0, k == H - 1)
            for k in range(CK):
                mm(pyb[:, 0:TG], wcc[:, k, :], sT[:, k, :], k == 0, k == CK - 1)
            act(ga, pga[:, 0:TG], AF.Sigmoid)
            act(gb, pgb[:, 0:TG], AF.Sigmoid)
            tt(ga, ga, pya[:, 0:TG], ALU.mult)
            tt(gb, gb, pyb[:, 0:TG], ALU.mult)
            tt(mT[:, dm, :], ga, gb, ALU.add)
        ckpt('d3')
        P.barrier()
        AR.pos = mark
        wo = [A16.alloc([DK, 512], "wo%d" % i_) for i_ in range(2)]
        xin = A32.alloc([D], "xin")
        stats = A32.alloc([8, 6], "stats")
        mv = A32.alloc([4], "mv")
        lg = A32.alloc([D], "lg")
        lb = A32.alloc([D], "lb")
        ldb(lg, ln1g_d, D)
        ldb(lb, ln1b_d, D)
        NDC = (D + 511) // 512
        x1b = A32.alloc([D], "x1b")
        xTst = A16.alloc([DK, 128], "xTst")
        rws = A32.alloc([DK, E], "rws")
        ld(rws, rw_d.re("(k p) e -> p k e", p=128))
        rbs = A32.alloc([E], "rbs")
        ldb(rbs, rb_d, E)
        x1T32 = A32.alloc([DK, 128], "x1T32")
        lgt = A32.alloc([E], "lgt")
        lg2 = A32.alloc([E], "lg2")
        gts = A32.alloc([E], "gts")
        mxr = A32.alloc([8], "mxr")
        sm = A32.alloc([4], "sm")
        for b in range(BPG):
            row0 = (tg * BPG + b) * 128
            ld(xin, xtok_d[row0:row0 + 128, :])
            for dc in range(NDC):
                cw = min(512, D - dc * 512)
                w = wo[dc % 2]
                if b == 0 or NDC > 2:
                    ldc(w[:, :, 0:cw], w_out_v[:, :, dc * 512:dc * 512 + cw])
                pm = banks[4 + dc % 2]
                for k in range(DK):
                    mm(pm[:, 0:cw], mT[:, k, b * 128:(b + 1) * 128], w[:, k, 0:cw], k == 0, k == DK - 1)
                stt(xin[:, dc * 512:dc * 512 + cw], xin[:, dc * 512:dc * 512 + cw], ALPHA, pm[:, 0:cw], ALU.mult, ALU.add)
                P.op("dve", lambda e, dc=dc, cw=cw, stats=stats, xin=xin: e.bn_stats(out=stats.ap[:, dc, :], in_=xin.ap[:, dc * 512:dc * 512 + cw]),
                     reads=[xin.b], writes=[stats.b])
            P.op("dve", lambda e, mv=mv, stats=stats, NDC=NDC: e.bn_aggr(out=mv.ap[:, 0:2], in_=stats.ap.rearrange("p a b -> p (a b)")[:, 0:NDC * 6]), reads=[stats.b], writes=[mv.b])
            act(mv[:, 2:3], mv[:, 1:2], AF.Sqrt, bias=eps_t)
            P.op("dve", lambda e, mv=mv: e.reciprocal(out=mv.ap[:, 3:4], in_=mv.ap[:, 2:3]), reads=[mv.b], writes=[mv.b])
            ts(x1b, xin, mv[:, 0:1], mv[:, 3:4], ALU.subtract, ALU.mult)
            tt(x1b, x1b, lg, ALU.mult)
            tt(x1b, x1b, lb, ALU.add)
            ckpt('d4')
            ld(x1_s[row0:row0 + 128, :], x1b)
            for k in range(DK):
                ptr = banks[k % 2]
                tr(ptr[:, 0:128], x1b[:, k * 128:(k + 1) * 128], ident)
                cp(x1T32[:, k, :], ptr[:, 0:128])
                act(xTst[:, k, :], x1T32[:, k, :], AF.Copy)
            ld(x1T_send_v[:, :, row0:row0 + 128], xTst)
            ckpt('d5')
            pr = banks[2]
            for k in range(DK):
                mm(pr[:, 0:E], x1T32[:, k, :], rws[:, k, :], k == 0, k == DK - 1)
            tt(lgt, pr[:, 0:E], rbs, ALU.add)
            P.op("dve", lambda e, mxr=mxr, lgt=lgt: e.max(out=mxr.ap, in_=lgt.ap), reads=[lgt.b], writes=[mxr.b])
            ts(lg2, lgt, mxr[:, 3:4], None, ALU.is_ge)
            ts(gts, lgt, mxr[:, 0:1], None, ALU.subtract)
            act(gts, gts, AF.Exp)
            tt(gts, gts, lg2, ALU.mult)
            red(sm[:, 0:1], gts, ALU.add)
            P.op("dve", lambda e, sm=sm: e.reciprocal(out=sm.ap[:, 1:2], in_=sm.ap[:, 0:1]), reads=[sm.b], writes=[sm.b])
            ts(gts, gts, sm[:, 1:2], None, ALU.mult)
            ld(gate_send[row0:row0 + 128, :], gts)

    ckpt('pD')
    for a_ in range(DK):
        ag8(x1T_send[a_ * 128:(a_ + 1) * 128, :], x1T_all[a_])
    ag8(gate_send, gate_all)
    x1T_all_v = [t_.re("(r p) n -> p r n", p=128) for t_ in x1T_all]
    hT_v = hT_s.re("(k p) n -> p k n", p=128)
    TT = min(512, NT)
    NTT = NT // TT
    FH = max(1, FK // 8)
    FC = FK // FH
    for el in range(EPC):
        for fh in range(FH):
            new_phase()
            wgs = A16.alloc([DK, FC * 128], "wgs")
            wus = A16.alloc([DK, FC * 128], "wus")
            ldc(wgs, wg_d[el].re("(k p) f -> p k f", p=128)[:, :, fh * FC * 128:(fh + 1) * FC * 128])
            ldc(wus, wu_d[el].re("(k p) f -> p k f", p=128)[:, :, fh * FC * 128:(fh + 1) * FC * 128])
            bgs = A32.alloc([FC], "bgs")
            bus = A32.alloc([FC], "bus")
            ld(bgs, bg_d[el][:, fh * FC:(fh + 1) * FC])
            ld(bus, bu_d[el][:, fh * FC:(fh + 1) * FC])
            xt = [A16.alloc([DK, TT], "xt%d" % i_) for i_ in range(2)]
            hst = [A16.alloc([TT], "hst%d" % i_) for i_ in range(2)]
            gg = A32.alloc([TT], "gg")
            uu = A32.alloc([TT], "uu")
            sgm = A32.alloc([TT], "sgm")
            n_ = 0
            for r in range(NCORES):
                for t in range(NTT):
                    xx = xt[n_ % 2]
                    for k in range(DK):
                        ld(xx[:, k, :], x1T_all_v[k][:, r, t * TT:(t + 1) * TT])
                    col0 = r * NT + t * TT
                    for fo in range(FC):
                        pg = banks[(2 * fo) % 4]
                        pu = banks[(2 * fo + 1) % 4]
                        for k in range(DK):
                            mm(pg[:, 0:TT], wgs[:, k, fo * 128:(fo + 1) * 128], xx[:, k, :], k == 0, k == DK - 1)
                        for k in range(DK):
                            mm(pu[:, 0:TT], wus[:, k, fo * 128:(fo + 1) * 128], xx[:, k, :], k == 0, k == DK - 1)
                        ts(gg, pg[:, 0:TT], bgs[:, fo:fo + 1], 7.0, ALU.add, ALU.min)
                        ts(uu, pu[:, 0:TT], bus[:, fo:fo + 1], 7.0, ALU.add, ALU.min)
                        ts(uu, uu, -7.0, 1.0, ALU.max, ALU.add)
                        act(sgm, gg, AF.Sigmoid, scale=1.702)
                        tt(gg, gg, sgm, ALU.mult, eng="pool")
                        hs = hst[fo % 2]
                        tt(hs, gg, uu, ALU.mult)
                        ld(hT_v[:, fh * FC + fo, col0:col0 + TT], hs)
                    n_ += 1
        new_phase()
        wds = A16.alloc([FK, D], "wds")
        ldc(wds, wd_d[el].re("(k p) n -> p k n", p=128))
        bdr = A32.alloc([D], "bdr")
        ldb(bdr, bd_d[el], D)
        selm = A32.alloc([EPC, E], "selm")
        ldb(selm.re("p a b -> p (a b)"), selm_d, EPC * E)
        ht = [A16.alloc([FK, 128], "ht%d" % i_) for i_ in range(2)]
        gcol = [A32.alloc([E], "gcol%d" % i_) for i_ in range(2)]
        gtmp = A32.alloc([E], "gtmp")
        gsel = A32.alloc([2], "gsel")
        yo = [A32.alloc([D], "yo%d" % i_) for i_ in range(2)]
        yp = [A32.alloc([D], "yp%d" % i_) for i_ in range(2)]
        NDC2 = (D + 511) // 512
        for tc in range(TALL // 128):
            hh_ = ht[tc % 2]
            ld(hh_, hT_v[:, :, tc * 128:(tc + 1) * 128])
            gc = gcol[tc % 2]
            ld(gc, gate_all[tc * 128:(tc + 1) * 128, :])
            tt(gtmp, gc, selm[:, el, :], ALU.mult)
            red(gsel[:, 0:1], gtmp, ALU.add)
            y = yo[tc % 2]
            if el > 0:
                ypv = yp[tc % 2]
                ld(ypv, part_s[tc * 128:(tc + 1) * 128, :])
            for dc in range(NDC2):
                cw = min(512, D - dc * 512)
                pm = banks[dc % 4]
                for k in range(FK):
                    mm(pm[:, 0:cw], hh_[:, k, :], wds[:, k, dc * 512:dc * 512 + cw], k == 0, k == FK - 1)
                tt(y[:, dc * 512:dc * 512 + cw], pm[:, 0:cw], bdr[:, dc * 512:dc * 512 + cw], ALU.add)
            if el > 0:
                stt(y, y, gsel[:, 0:1], ypv, ALU.mult, ALU.add)
            else:
                ts(y, y, gsel[:, 0:1], None, ALU.mult)
            ld(part_s[tc * 128:(tc + 1) * 128, :], y)

    ckpt('pE')
    rs8(part_s, f_s)

    new_phase()
    lg = A32.alloc([D], "lg2g")
    lb = A32.alloc([D], "lg2b")
    ldb(lg, ln2g_d, D)
    ldb(lb, ln2b_d, D)
    xa = [A32.alloc([D], "xa%d" % i_) for i_ in range(2)]
    fa = [A32.alloc([D], "fa%d" % i_) for i_ in range(2)]
    stats = A32.alloc([8, 6], "stats2")
    mv = A32.alloc([4], "mv2")
    NDC = (D + 511) // 512
    for b in range(NB):
        xv = xa[b % 2]
        fv = fa[b % 2]
        ld(xv, x1_s[b * 128:(b + 1) * 128, :])
        ld(fv, f_s[b * 128:(b + 1) * 128, :])
        stt(xv, xv, ALPHA, fv, ALU.mult, ALU.add)
        for dc in range(NDC):
            cw = min(512, D - dc * 512)
            P.op("dve", lambda e, dc=dc, cw=cw, xv=xv, stats=stats: e.bn_stats(out=stats.ap[:, dc, :], in_=xv.ap[:, dc * 512:dc * 512 + cw]),
                 reads=[xv.b], writes=[stats.b])
        P.op("dve", lambda e, mv=mv, stats=stats, NDC=NDC: e.bn_aggr(out=mv.ap[:, 0:2], in_=stats.ap.rearrange("p a b -> p (a b)")[:, 0:NDC * 6]), reads=[stats.b], writes=[mv.b])
        act(mv[:, 2:3], mv[:, 1:2], AF.Sqrt, bias=eps_t)
        P.op("dve", lambda e, mv=mv: e.reciprocal(out=mv.ap[:, 3:4], in_=mv.ap[:, 2:3]), reads=[mv.b], writes=[mv.b])
        ts(fv, xv, mv[:, 0:1], mv[:, 3:4], ALU.subtract, ALU.mult)
        tt(fv, fv, lg, ALU.mult)
        tt(fv, fv, lb, ALU.add)
        ld(out_d[b * 128:(b + 1) * 128, :], fv)
    P.barrier()
    P.finish()
    return nc, c


def _tables(c, r):
    NQ, NB, H = c["NQ"], c["NB"], c["H"]
    slopes = np.exp2(-8.0 * np.arange(1, H + 1, dtype=np.float64) / H)
    k = np.arange(128, dtype=np.float64)
    u = np.arange(NQ + 3)
    delta = u - 3 + r
    rel = (-128.0 * delta[None, :] + k[:, None] - 64.0)
    al = slopes[None, :, None] * rel[:, None, :]
    al = np.where(delta[None, None, :] >= 0, al, NEG)
    alibi = al.reshape(128, H * (NQ + 3)).astype(np.float32)
    ki = np.arange(128)[:, None]
    qi = np.arange(128)[None, :]
    wm = np.zeros((128, 8, 128), np.float32)
    for dj in range(8):
        d = r + 4 - dj
        if d == 0:
            wm[:, dj, :] = (ki <= qi)
        elif 1 <= d <= 3:
            wm[:, dj, :] = 1.0
        elif d == 4:
            wm[:, dj, :] = (ki > qi)
    ca = np.zeros((128, 4, 128), np.float32)
    for dj in range(4):
        d = r - dj
        if d > 0:
            ca[:, dj, :] = 1.0
        elif d == 0:
            ca[:, dj, :] = (ki <= qi)
    MC = 32 * NB
    m = np.arange(MC)
    npr = m - 32 * (NB - 1) - 8 * r
    dist = np.arange(128)[:, None] - 16.0 * npr[None, :] - 31.0
    cb = -slopes[None, :, None] * dist[:, None, :]
    cb = np.where(dist[:, None, :] >= 0, cb, NEG)
    cmpb = cb.reshape(128, H * MC).astype(np.float32)
    mm_ = np.arange(8 * NB)
    jp = mm_ - 8 * (NB - 1) - 2 * r
    q = np.arange(128)[:, None]
    curp = (q >= 64).astype(np.int64)
    valid = (64 * jp[None, :] <= q)
    forced = (jp[None, :] == curp) | (jp[None, :] == curp - 1)
    selF = np.where(valid, np.where(forced, 1.0e4, 0.0), -1.0e9).astype(np.float32)
    selV = valid.astype(np.float32)
    return dict(alibi_tab=alibi, wmask=wm.reshape(128, 8 * 128).astype(ml_dtypes.bfloat16),
                camask=ca.reshape(128, 4 * 128).astype(ml_dtypes.bfloat16), cmpb=cmpb, selF=selF, selV=selV)


def make_in_maps(c, inp):
    S, D, H, E, CH = c["S"], c["D"], c["H"], c["E"], c["CH"]
    NB, NT, EPC = c["NB"], c["NT"], c["EPC"]
    f32 = lambda a: np.ascontiguousarray(a, dtype=np.float32)
    x = inp["x"]
    shared = dict(
        cmp_peT=f32(inp["cmp_pe"][0].transpose(0, 2, 1)),
        cmp_w1=f32(inp["cmp_w1"][0]),
        cmp_b1=f32(inp["cmp_b1"][0].reshape(2, 128, 1)),
        cmp_w2=f32(inp["cmp_w2"][0]),
        cmp_b2=f32(inp["cmp_b2"][0].reshape(2, 128, 1)),
        cmp_b2r=f32(inp["cmp_b2"][0].reshape(2, 1, 128)),
        conv_wT=f32(inp["conv_w"][0].T),
        conv_b=f32(inp["conv_b"][0].reshape(CH // 128, 128).T),
        conv_ln_g=f32(inp["conv_ln_g"][0].reshape(CH // 128, 128).T),
        conv_ln_b=f32(inp["conv_ln_b"][0].reshape(CH // 128, 128).T),
        ln1_g=f32(inp["ln1_g"][0].reshape(1, D)), ln1_b=f32(inp["ln1_b"][0].reshape(1, D)),
        ln2_g=f32(inp["ln2_g"][0].reshape(1, D)), ln2_b=f32(inp["ln2_b"][0].reshape(1, D)),
        router_w=f32(inp["router_w"][0]), router_b=f32(inp["router_b"][0].reshape(1, E)),
        ident=np.eye(128, dtype=np.float32),
    )
    maps = []
    for k in range(NCORES):
        b, r = k // CPB, k % CPB
        xT = np.zeros((D, NB * BW), np.float32)
        xtok = np.zeros((NT, D), np.float32)
        for i in range(NB):
            cblk = CPB * i + r
            t0 = 128 * cblk - HALO
            lo = max(t0, 0)
            xT[:, i * BW + (lo - t0):(i + 1) * BW] = x[b, lo:128 * cblk + 128].T
            xtok[i * 128:(i + 1) * 128] = x[b, 128 * cblk:128 * cblk + 128]
        m = dict(shared)
        m.update(_tables(c, r))
        selm = np.zeros((EPC, E), np.float32)
        for el in range(EPC):
            selm[el, k * EPC + el] = 1.0
        sh = lambda w: f32(w[k * (w.shape[0] // 8):(k + 1) * (w.shape[0] // 8)])
        m.update(
            xT=xT, xtok=xtok,
            w_in=f32(inp["w_in"][0]), w_nsa=f32(inp["w_nsa_proj"][0]),
            w_cp=f32(inp["w_conv_proj"][0]), w_out=f32(inp["w_out"][0]),
            w_gate=f32(inp["w_gate"][0][k * EPC:(k + 1) * EPC]),
            w_up=f32(inp["w_up"][0][k * EPC:(k + 1) * EPC]),
            w_down=f32(inp["w_down"][0][k * EPC:(k + 1) * EPC]),
            b_gate=f32(inp["b_gate"][0][k * EPC:(k + 1) * EPC].reshape(EPC, -1, 128).transpose(0, 2, 1)),
            b_up=f32(inp["b_up"][0][k * EPC:(k + 1) * EPC].reshape(EPC, -1, 128).transpose(0, 2, 1)),
            b_down=f32(inp["b_down"][0][k * EPC:(k + 1) * EPC].reshape(EPC, 1, D)),
            selm=selm.reshape(1, EPC * E),
        )
        maps.append(m)
    return maps


def run_cfg(cfg, inp, trace=False, stop_after=None):
    nc, c = build_program(cfg, stop_after)
    maps = make_in_maps(c, inp)
    res = run_bass_kernel_spmd(nc, maps, core_ids=list(range(NCORES)))
    S, D, NB = c["S"], c["D"], c["NB"]
    out = np.zeros((2, S, D), np.float32)
    for k in range(NCORES):
        b, r = k // CPB, k % CPB
        o = res.results[k]["out"]
        for i in range(NB):
            cblk = CPB * i + r
            out[b, 128 * cblk:128 * cblk + 128] = o[i * 128:(i + 1) * 128]
    return out


def kernel(**inputs):
    inp = {k: np.asarray(v) for k, v in inputs.items()}
    return run_cfg(CFG_FULL, inp)
```

```python
import contextlib
import math
import numpy as np
import ml_dtypes
import concourse.bass as bass
import concourse.mybir as mybir
from concourse.bass_utils import run_bass_kernel_spmd

F32 = mybir.dt.float32
BF16 = mybir.dt.bfloat16
AF = mybir.ActivationFunctionType
ALU = mybir.AluOpType
AX = mybir.AxisListType

NCORES = 8
CPB = 4
G = 2
DH = 128
HALO = 30
BW = 128 + HALO
NEG = -30000.0
TINY = 1e-30

CFG_FULL = dict(S=8192, D=2048, H=16, TOPN=16, E=32, DFF=2048, CH=1024)


class Buf:
    __slots__ = ("name", "w", "r")

    def __init__(self, name=""):
        self.name = name
        self.w = {}
        self.r = {}


class V:
    __slots__ = ("ap", "b")

    def __init__(self, ap, b):
        self.ap = ap
        self.b = b

    def __getitem__(self, idx):
        return V(self.ap[idx], self.b)

    def re(self, pat, **kw):
        return V(self.ap.rearrange(pat, **kw), self.b)


class Prog:
    ENGS = ("pe", "act", "dve", "pool", "sp")

    def __init__(self, nc, n_ring=8):
        self.nc = nc
        self.es = contextlib.ExitStack()
        self.lists = {e: [] for e in self.ENGS}
        self.sems = {}
        self.cnt = {}
        for e in ("pe", "act", "dve", "pool"):
            self.sems[e] = self.es.enter_context(nc.semaphore("s_" + e))
            self.cnt[e] = 0
        self.ring = {}
        self.ring_pos = {}
        for q in ("sp", "pool"):
            self.ring[q] = []
            for i in range(n_ring):
                k = "d_%s%d" % (q, i)
                self.sems[k] = self.es.enter_context(nc.semaphore(k))
                self.cnt[k] = 0
                self.ring[q].append(k)
            self.ring_pos[q] = 0
        self.sems["cc"] = self.es.enter_context(nc.semaphore("s_cc"))
        self.cnt["cc"] = 0
        self.waited = {e: {} for e in self.ENGS}
        self.n_inst = 0

    def _need(self, eng, k, v, waits):
        if eng == "pe" and k == "pe":
            return
        if self.waited[eng].get(k, 0) >= v:
            return
        if waits.get(k, 0) < v:
            waits[k] = v

    def _deps(self, eng, reads, writes):
        waits = {}
        for b in reads:
            for k, v in b.w.items():
                self._need(eng, k, v, waits)
        for b in writes:
            for k, v in b.w.items():
                self._need(eng, k, v, waits)
            for k, v in b.r.items():
                self._need(eng, k, v, waits)
        for k, v in waits.items():
            self.waited[eng][k] = v
        return list(waits.items())

    def _commit(self, tok, reads, writes):
        k, v = tok
        for b in reads:
            if b.r.get(k, 0) < v:
                b.r[k] = v
        for b in writes:
            b.w[k] = v
            b.r = {}

    def op(self, eng, fn, reads=(), writes=()):
        waits = self._deps(eng, reads, writes)
        self.cnt[eng] += 1
        tok = (eng, self.cnt[eng])
        self.lists[eng].append(("op", fn, waits, eng, self.cnt[eng]))
        self._commit(tok, reads, writes)
        self.n_inst += 1

    def dma(self, q, fn, reads=(), writes=()):
        pos = self.ring_pos[q]
        self.ring_pos[q] = (pos + 1) % len(self.ring[q])
        k = self.ring[q][pos]
        waits = self._deps(q, reads, writes)
        prev = self.cnt[k]
        if prev > 0 and self.waited[q].get(k, 0) < prev:
            self.waited[q][k] = prev
            waits = waits + [(k, prev)]
        self.cnt[k] += 16
        tok = (k, self.cnt[k])
        self.lists[q].append(("dma", fn, waits, k, 16))
        self._commit(tok, reads, writes)
        self.n_inst += 1

    def collective(self, fn, reads=(), writes=()):
        waits = self._deps("pool", reads, writes)
        prev = self.cnt["cc"]
        if prev > 0 and self.waited["pool"].get("cc", 0) < prev:
            self.waited["pool"]["cc"] = prev
            waits = waits + [("cc", prev)]
        self.cnt["cc"] += 1
        tok = ("cc", self.cnt["cc"])
        self.lists["pool"].append(("dma", fn, waits, "cc", 1))
        self._commit(tok, reads, writes)

    def barrier(self):
        for eng in self.ENGS:
            waits = []
            for k, v in self.cnt.items():
                if v > 0 and self.waited[eng].get(k, 0) < v and not (eng == "pe" and k == "pe"):
                    self.waited[eng][k] = v
                    waits.append((k, v))
            self.lists[eng].append(("bar", None, waits, None, 0))

    def finish(self):
        nc = self.nc
        L = self.lists
        comp = ("pe", "act", "dve", "pool")
        W = {e: set() for e in comp}
        for eng in self.ENGS:
            for _, _, waits, _, _ in L[eng]:
                for k, v in waits:
                    if k in W:
                        W[k].add(v)
        rank = {e: {v: i + 1 for i, v in enumerate(sorted(W[e]))} for e in comp}
        self.n_incs = {e: len(W[e]) for e in comp}
        sems = self.sems

        def run(e, recs):
            for kind, fn, waits, key, n in recs:
                for k, v in waits:
                    e.wait_ge(sems[k], rank[k][v] if k in rank else v)
                if kind == "op":
                    r = fn(e)
                    if n in W[key]:
                        r.then_inc(sems[key], 1)
                elif kind == "dma":
                    fn(e).then_inc(sems[key], n)

        with nc.Block() as block:
            @block.tensor
            def _(e):
                run(e, L["pe"])

            @block.scalar
            def _(e):
                run(e, L["act"])

            @block.vector
            def _(e):
                run(e, L["dve"])

            @block.gpsimd
            def _(e):
                run(e, L["pool"])

            @block.sync
            def _(e):
                run(e, L["sp"])
        self.es.close()


class Arena:
    def __init__(self, P, name, ncols):
        self.t = P.es.enter_context(P.nc.sbuf_tensor(name, [128, ncols], F32))
        self.n = ncols
        self.pos = 0
        self.name = name

    def reset(self):
        self.pos = 0

    def alloc(self, shape, dtype, name=""):
        ne = int(np.prod(shape))
        n32 = ne if dtype == F32 else (ne + 1) // 2
        n32 = (n32 + 7) // 8 * 8
        assert self.pos + n32 <= self.n, "arena overflow: %s needs %d at %d/%d" % (name, n32, self.pos, self.n)
        ap = self.t[:, self.pos:self.pos + n32]
        self.pos += n32
        if dtype != F32:
            ap = ap.bitcast(dtype)
        ap = ap[:, 0:ne]
        if len(shape) == 2:
            ap = ap.rearrange("p (a b) -> p a b", b=shape[1])
        elif len(shape) == 3:
            ap = ap.rearrange("p (a b c) -> p a b c", b=shape[1], c=shape[2])
        return V(ap, Buf(name))


def derive(cfg):
    c = dict(cfg)
    S, D, H, E = c["S"], c["D"], c["H"], c["E"]
    c["NQ"] = S // 128
    c["NB"] = c["NQ"] // CPB
    c["NT"] = c["NB"] * 128
    c["HPG"] = H // G
    c["NCMP"] = (S - 32) // 16 + 1
    c["NSEL"] = S // 64
    c["DK"] = D // 128
    c["EPC"] = E // NCORES
    c["TALL"] = NCORES * c["NT"]
    off = {}
    o = 0
    for nm, w in (("q", H * DH), ("kc", G * DH), ("vc", G * DH), ("ks", G * DH), ("vs", G * DH),
                  ("kw", G * DH), ("vw", G * DH), ("gn", H * 3), ("glu", 2 * c["CH"]), ("mrg", 2 * D)):
        off[nm] = o
        o += w
    c["off"] = off
    c["IN_DIM"] = o
    return c


class _Stop(Exception):
    pass


def build_program(cfg, stop_after=None):
    try:
        return _build_program(cfg, stop_after)
    except _Stop as e:
        return e.args


def _build_program(cfg, stop_after=None):
    c = derive(cfg)
    S, D, H, E, DFF, CH, TOPN = c["S"], c["D"], c["H"], c["E"], c["DFF"], c["CH"], c["TOPN"]
    NQ, NB, NT, HPG, NCMP, NSEL, DK, EPC, TALL = (c["NQ"], c["NB"], c["NT"], c["HPG"], c["NCMP"],
                                                   c["NSEL"], c["DK"], c["EPC"], c["TALL"])
    off, IN_DIM = c["off"], c["IN_DIM"]
    CK = CH // 128
    FK = DFF // 128
    scale = 1.0 / math.sqrt(DH)
    ALPHA = 2.0 ** 0.25
    NTG = max(1, NB // 4)
    BPG = NB // NTG
    TG = BPG * 128
    assert TG <= 512

    nc = bass.Bass("TRN2", target_bir_lowering=False)
    P = Prog(nc)

    def din(name, shape, dt=F32):
        return V(nc.dram_tensor(name, list(shape), dt, kind="ExternalInput").ap(), Buf(name))

    def dtmp(name, shape, dt=F32):
        return V(nc.dram_tensor(name, list(shape), dt).ap(), Buf(name))

    xT_d = din("xT", [D, NB * BW])
    xtok_d = din("xtok", [NT, D])
    w_in_f = din("w_in", [D, IN_DIM])
    w_nsa_f = din("w_nsa", [H * DH, D])
    w_cp_f = din("w_cp", [CH, D])
    w_out_f = din("w_out", [D, D])
    peT_d = din("cmp_peT", [2, 128, 32])
    w1_d = din("cmp_w1", [2, 32 * 128, 128])
    b1_d = din("cmp_b1", [2, 128, 1])
    w2_d = din("cmp_w2", [2, 128, 128])
    b2_d = din("cmp_b2", [2, 128, 1])
    b2r_d = din("cmp_b2r", [2, 1, 128])
    convw_d = din("conv_wT", [CH, 31])
    convb_d = din("conv_b", [128, CH // 128])
    clng_d = din("conv_ln_g", [128, CH // 128])
    clnb_d = din("conv_ln_b", [128, CH // 128])
    ln1g_d = din("ln1_g", [1, D])
    ln1b_d = din("ln1_b", [1, D])
    ln2g_d = din("ln2_g", [1, D])
    ln2b_d = din("ln2_b", [1, D])
    rw_d = din("router_w", [D, E])
    rb_d = din("router_b", [1, E])
    wg_d = din("w_gate", [EPC, D, DFF])
    wu_d = din("w_up", [EPC, D, DFF])
    wd_d = din("w_down", [EPC, DFF, D])
    bg_d = din("b_gate", [EPC, 128, DFF // 128])
    bu_d = din("b_up", [EPC, 128, DFF // 128])
    bd_d = din("b_down", [EPC, 1, D])
    ident_d = din("ident", [128, 128])
    alibi_d = din("alibi_tab", [128, H * (NQ + 3)])
    wmask_d = din("wmask", [128, 8 * 128], BF16)
    camask_d = din("camask", [128, 4 * 128], BF16)
    MC = 32 * NB
    cmpb_d = din("cmpb", [128, H * MC])
    selF_d = din("selF", [128, 8 * NB])
    selV_d = din("selV", [128, 8 * NB])
    selm_d = din("selm", [1, EPC * E])
    out_d = V(nc.dram_tensor("out", [NT, D], F32, kind="ExternalOutput").ap(), Buf("out"))

    qT_s = dtmp("qT_s", [H * 128, NT], BF16)
    kT_send = dtmp("kT_send", [4 * G * 128, NT], BF16)
    kT_all = [dtmp("kT_all%d" % a, [CPB * 128, NT], BF16) for a in range(4 * G)]
    v_send = dtmp("v_send", [NT, 2 * G * 128], BF16)
    NVC = max(1, NT // 1024)
    NBV = NB // NVC
    v_all = [dtmp("v_all%d" % a, [CPB * NBV * 128, 2 * G * 128], BF16) for a in range(NVC)]
    onT_s = dtmp("onT_s", [H * 128, NT], BF16)
    x1_s = dtmp("x1_s", [NT, D])
    x1T_send = dtmp("x1T_send", [D, NT], BF16)
    x1T_all = [dtmp("x1T_all%d" % a, [NCORES * 128, NT], BF16) for a in range(DK)]
    gate_send = dtmp("gate_send", [NT, E])
    gate_all = dtmp("gate_all", [TALL, E])
    hT_s = dtmp("hT_s", [DFF, TALL], BF16)
    part_s = dtmp("part_s", [TALL, D])
    f_s = dtmp("f_s", [NT, D])

    AR = Arena(P, "arena", 39 * 1024)

    class _A:
        def __init__(self, dt):
            self.dt = dt

        def alloc(self, shape, name=""):
            return AR.alloc(shape, self.dt, name)

    A16 = _A(BF16)
    A32 = _A(F32)
    banks = [V(P.es.enter_context(nc.psum_tensor("pb%d" % i, [128, 512], F32))[:], Buf("pb%d" % i)) for i in range(8)]

    def ckpt(name):
        if stop_after == name:
            P.barrier()
            P.finish()
            raise _Stop(nc, c)

    def new_phase():
        P.barrier()
        AR.reset()

    def mm(out, lhsT, rhs, start, stop):
        P.op("pe", lambda e: e.matmul(out.ap, lhsT=lhsT.ap, rhs=rhs.ap, start=start, stop=stop),
             reads=[lhsT.b, rhs.b], writes=[out.b])

    def tr(out, in_, idn):
        P.op("pe", lambda e: e.transpose(out.ap, in_.ap, idn.ap), reads=[in_.b, idn.b], writes=[out.b])

    def act(out, in_, func, bias=None, scale=None, accum=None, eng="act"):
        kw = {}
        rd = [in_.b]
        wr = [out.b]
        if bias is not None:
            if isinstance(bias, V):
                kw["bias"] = bias.ap
                rd.append(bias.b)
            else:
                kw["bias"] = bias
        if scale is not None:
            if isinstance(scale, V):
                kw["scale"] = scale.ap
                rd.append(scale.b)
            else:
                kw["scale"] = scale
        if accum is not None:
            kw["accum_out"] = accum.ap
            wr.append(accum.b)
        P.op("act", lambda e: e.activation(out=out.ap, in_=in_.ap, func=func, **kw), reads=rd, writes=wr)

    def ts(out, in0, s1, s2, op0, op1=None, eng="dve"):
        rd = [in0.b]
        a1 = s1
        a2 = s2
        if isinstance(s1, V):
            a1 = s1.ap
            rd.append(s1.b)
        if isinstance(s2, V):
            a2 = s2.ap
            rd.append(s2.b)
        if op1 is None:
            P.op(eng, lambda e: e.tensor_scalar(out=out.ap, in0=in0.ap, scalar1=a1, scalar2=None, op0=op0),
                 reads=rd, writes=[out.b])
        else:
            P.op(eng, lambda e: e.tensor_scalar(out=out.ap, in0=in0.ap, scalar1=a1, scalar2=a2, op0=op0, op1=op1),
                 reads=rd, writes=[out.b])

    def stt(out, in0, s, in1, op0, op1):
        rd = [in0.b, in1.b]
        a = s
        if isinstance(s, V):
            a = s.ap
            rd.append(s.b)
        P.op("dve", lambda e: e.scalar_tensor_tensor(out=out.ap, in0=in0.ap, scalar=a, in1=in1.ap, op0=op0, op1=op1),
             reads=rd, writes=[out.b])

    def tt(out, in0, in1, op, eng="dve"):
        P.op(eng, lambda e: e.tensor_tensor(out=out.ap, in0=in0.ap, in1=in1.ap, op=op),
             reads=[in0.b, in1.b], writes=[out.b])

    def cp(out, in_, eng="dve"):
        P.op(eng, lambda e: e.tensor_copy(out=out.ap, in_=in_.ap), reads=[in_.b], writes=[out.b])

    def red(out, in_, op, negate=False):
        P.op("dve", lambda e: e.tensor_reduce(out=out.ap, in_=in_.ap, axis=AX.X, op=op, negate=negate),
             reads=[in_.b], writes=[out.b])

    def memset(out, val, eng="dve"):
        P.op(eng, lambda e: e.memset(out.ap, val), writes=[out.b])

    def ld(out, in_, q="sp"):
        P.dma(q, lambda e: e.dma_start(out=out.ap, in_=in_.ap), reads=[in_.b], writes=[out.b])

    def ldc(out, in_):
        P.dma("pool", lambda e: e.dma_start(out=out.ap, in_=in_.ap), reads=[in_.b], writes=[out.b])

    def ldb(out, in_, n):
        P.dma("sp", lambda e: e.dma_start(out=out.ap, in_=in_.ap.to_broadcast([128, n])), reads=[in_.b], writes=[out.b])

    grp4 = [[0, 1, 2, 3], [4, 5, 6, 7]]
    pairs = [[0, 4], [1, 5], [2, 6], [3, 7]]
    cc_n = [0]

    def ag8(src, dst):
        rows, cols = src.ap.shape
        cc_n[0] += 1
        mid = V(nc.dram_tensor("ccmid%d" % cc_n[0], [4 * rows, cols], src.ap.dtype).ap(), Buf("ccmid"))
        P.collective(lambda e: e.collective_compute("AllGather", ALU.bypass, replica_groups=grp4,
                                                    ins=[src.ap], outs=[mid.ap]), reads=[src.b], writes=[mid.b])
        P.collective(lambda e: e.collective_compute("AllGather", ALU.bypass, replica_groups=pairs,
                                                    ins=[mid.ap], outs=[dst.ap]), reads=[mid.b], writes=[dst.b])

    def rs8(src, dst):
        rows, cols = src.ap.shape
        cc_n[0] += 1
        mid = V(nc.dram_tensor("ccmid%d" % cc_n[0], [rows // 2, cols], src.ap.dtype).ap(), Buf("ccmid"))
        P.collective(lambda e: e.collective_compute("ReduceScatter", ALU.add, replica_groups=pairs,
                                                    ins=[src.ap], outs=[mid.ap]), reads=[src.b], writes=[mid.b])
        P.collective(lambda e: e.collective_compute("ReduceScatter", ALU.add, replica_groups=grp4,
                                                    ins=[mid.ap], outs=[dst.ap]), reads=[mid.b], writes=[dst.b])


    ckpt('p0')
    def const_tile(name, shape, dt):
        return V(P.es.enter_context(nc.sbuf_tensor(name, list(shape), dt))[:], Buf(name))

    ident = const_tile("ident_sb", [128, 128], F32)
    ld(ident, ident_d)
    identb = const_tile("identb_sb", [128, 128], BF16)
    cp(identb, ident)
    gsig = const_tile("gsig", [128, NB, H * 3], F32)
    ones_f = const_tile("ones_f", [128, 128], F32)
    memset(ones_f, 1.0)
    eps_t = const_tile("eps_t", [128, 1], F32)
    memset(eps_t, 1e-5)

    w_in_v = w_in_f.re("(k p) n -> p k n", p=128)

    new_phase()
    xT_v = xT_d.re("(k p) n -> p k n", p=128)
    xg = [A16.alloc([DK, BPG * BW], "xg%d" % i) for i in range(2)]
    wt = [A16.alloc([DK, 512], "wt%d" % i) for i in range(2)]
    stg = [A16.alloc([512], "stg%d" % i) for i in range(2)]
    stg32 = A32.alloc([512], "stg32")
    wcount = [0]

    def load_w_cols(c0, n):
        t = wt[wcount[0] % 2]
        wcount[0] += 1
        ldc(t[:, :, 0:n], w_in_v[:, :, c0:c0 + n])
        return t

    kT_send_v = kT_send.re("(a p) n -> p a n", p=128)
    qT_v = qT_s.re("(a p) n -> p a n", p=128)
    ktypes = ("kc", "vc", "ks", "kw")
    for tg in range(NTG):
        x = xg[tg % 2]
        ldc(x, xT_v[:, :, tg * BPG * BW:(tg + 1) * BPG * BW])
        x3 = x.re("p k (b w) -> p k b w", w=BW)
        fm_cols = [(off["q"] + h * 128, ("q", h)) for h in range(H)]
        for ti, nm in enumerate(ktypes):
            for g in range(G):
                fm_cols.append((off[nm] + g * 128, ("k", ti * G + g)))
        for ci in range(0, len(fm_cols), 4):
            chunk = fm_cols[ci:ci + 4]
            contiguous = all(chunk[j][0] == chunk[0][0] + 128 * j for j in range(len(chunk)))
            for j, (c0, dest) in enumerate(chunk):
                if contiguous:
                    if j == 0:
                        w = load_w_cols(chunk[0][0], 128 * len(chunk))
                    wsl = w[:, :, j * 128:(j + 1) * 128]
                else:
                    w = load_w_cols(c0, 128)
                    wsl = w[:, :, 0:128]
                pb = banks[(ci + j) % 2]
                po = pb[:, 0:TG].re("p (b w) -> p b w", w=128)
                for k in range(DK):
                    mm(po, wsl[:, k, :], x3[:, k, :, HALO:BW], k == 0, k == DK - 1)
                st = stg[(ci + j) % 2]
                cp(st[:, 0:TG], pb[:, 0:TG], eng="dve" if j % 2 == 0 else "dve")
                if dest[0] == "q":
                    ld(qT_v[:, dest[1], tg * TG:(tg + 1) * TG], st[:, 0:TG])
                else:
                    ld(kT_send_v[:, dest[1], tg * TG:(tg + 1) * TG], st[:, 0:TG])
        wv = []
        for nm in ("vs", "vw"):
            wv.append(load_w_cols(off[nm], G * 128))
            for b in range(BPG):
                pb = banks[2 + b % 2]
                for k in range(DK):
                    mm(pb[:, 0:G * 128], x3[:, k, b, HALO:BW], wv[-1][:, k, 0:G * 128], k == 0, k == DK - 1)
                st = stg[b % 2]
                cp(st[:, 0:G * 128], pb[:, 0:G * 128])
                tsel = 0 if nm == "vs" else 1
                row0 = (tg * BPG + b) * 128
                ld(v_send[row0:row0 + 128, tsel * G * 128:(tsel + 1) * G * 128], st[:, 0:G * 128])
        wgn = load_w_cols(off["gn"], H * 3)
        for b in range(BPG):
            pb = banks[2 + b % 2]
            for k in range(DK):
                mm(pb[:, 0:H * 3], x3[:, k, b, HALO:BW], wgn[:, k, 0:H * 3], k == 0, k == DK - 1)
            act(gsig[:, tg * BPG + b, :], pb[:, 0:H * 3], AF.Sigmoid)

    ckpt('pA0')
    for a_ in range(4 * G):
        P.collective(lambda e, a_=a_: e.collective_compute("AllGather", ALU.bypass, replica_groups=grp4,
                                                           ins=[kT_send.ap[a_ * 128:(a_ + 1) * 128, :]],
                                                           outs=[kT_all[a_].ap]),
                     reads=[kT_send.b], writes=[kT_all[a_].b])
    for a_ in range(NVC):
        P.collective(lambda e, a_=a_: e.collective_compute("AllGather", ALU.bypass, replica_groups=grp4,
                                                           ins=[v_send.ap[a_ * NBV * 128:(a_ + 1) * NBV * 128, :]],
                                                           outs=[v_all[a_].ap]),
                     reads=[v_send.b], writes=[v_all[a_].b])

    ckpt('pA')
    kT_all_v = [t_.re("(r p) n -> p r n", p=128) for t_ in kT_all]
    v_all_v = [t_.re("(r i p) c -> p r i c", p=128, i=NBV) for t_ in v_all]
    onT_v = onT_s.re("(a p) n -> p a n", p=128)
    NCH = (NCMP + 127) // 128
    HB = min(4, HPG)
    KcT = const_tile("KcT", [128, 512], BF16)
    Vc = const_tile("Vc", [128, NCH, 130], BF16)

    for g in range(G):
        new_phase()
        Xc = A16.alloc([S], "Xc")
        w1s = A16.alloc([32, 128], "w1s")
        w2s = A16.alloc([128], "w2s")
        peT = A16.alloc([32], "peT")
        hid = A16.alloc([512], "hid")
        b1s = A32.alloc([1], "b1s")
        b2s = A32.alloc([1], "b2s")
        b2r = A32.alloc([128], "b2r")
        pbias = A32.alloc([1], "pbias")
        gx = A32.alloc([512], "gx")
        gy = A32.alloc([512], "gy")
        ckpt('c00')
        cp(Vc[:, :, 128:130], ones_f[:, 0:2 * NCH].re("p (a b) -> p a b", b=2))
        ckpt('c0')
        for t in range(2):
            d4 = Xc.re("p (i r q) -> p i r q", r=CPB, q=128)
            for r in range(CPB):
                ld(d4[:, :, r, :], kT_all_v[t * G + g][:, r, :].re("p (i q) -> p i q", q=128))
            ckpt('c0a')
            ldc(w1s, w1_d[t].re("(l p) o -> p l o", p=128))
            ldc(w2s, w2_d[t])
            ldc(peT, peT_d[t])
            ckpt('c0b')
            ld(b1s, b1_d[t])
            ld(b2s, b2_d[t])
            ldb(b2r, b2r_d[t], 128)
            ckpt('c1')
            ph = banks[0]
            pp = banks[1]
            for l in range(32):
                mm(ph[:, 0:NCMP], w1s[:, l, :], Xc[:, l:l + 16 * (NCMP - 1) + 1:16], l == 0, l == 31)
            ckpt('c2')
            for l in range(32):
                mm(pp[:, 0:1], w1s[:, l, :], peT[:, l:l + 1], l == 0, l == 31)
            ckpt('c3')
            tt(pbias, pp[:, 0:1], b1s, ALU.add)
            act(gx[:, 0:NCMP], ph[:, 0:NCMP], AF.Identity, bias=pbias)
            tt(gy[:, 0:NCMP], gx[:, 0:NCMP], gx[:, 0:NCMP], ALU.mult)
            ts(gy[:, 0:NCMP], gy[:, 0:NCMP], 0.044715, 1.0, ALU.mult, ALU.add)
            tt(gy[:, 0:NCMP], gy[:, 0:NCMP], gx[:, 0:NCMP], ALU.mult)
            act(gy[:, 0:NCMP], gy[:, 0:NCMP], AF.Tanh, scale=0.7978845608028654)
            stt(gy[:, 0:NCMP], gy[:, 0:NCMP], 1.0, gx[:, 0:NCMP], ALU.add, ALU.mult)
            ts(hid[:, 0:NCMP], gy[:, 0:NCMP], 0.5, None, ALU.mult)
            ckpt('c4')
            if t == 0:
                mm(pp[:, 0:NCMP], w2s, hid[:, 0:NCMP], True, True)
                act(KcT[:, 0:NCMP], pp[:, 0:NCMP], AF.Identity, bias=b2s)
            else:
                for ch in range(NCH):
                    n0 = ch * 128
                    nn = min(128, NCMP - n0)
                    mm(pp[0:nn, 0:128], hid[:, n0:n0 + nn], w2s, True, True)
                    tt(Vc[0:nn, ch, 0:128], pp[0:nn, 0:128], b2r[0:nn, :], ALU.add)
        ckpt('pCc')
        new_phase()
        KsT = A16.alloc([S], "KsT")
        KwT = A16.alloc([S], "KwT")
        Vs = A16.alloc([NQ, 130], "Vs")
        Vw = A16.alloc([NQ, 130], "Vw")
        QTb = [A16.alloc([HPG, 128], "QT%d" % i_) for i_ in range(2)]
        wmask = A16.alloc([8, 128], "wmask")
        camask = A16.alloc([4, 128], "camask")
        alibi = A32.alloc([H, NQ + 3], "alibi")
        cmpb = A32.alloc([HPG, MC], "cmpb")
        selF = A32.alloc([8 * NB], "selF")
        selV = A32.alloc([8 * NB], "selV")
        ld(wmask, wmask_d.re("p (a b) -> p a b", b=128))
        ld(camask, camask_d.re("p (a b) -> p a b", b=128))
        ld(alibi, alibi_d.re("p (a b) -> p a b", b=NQ + 3))
        ld(cmpb, cmpb_d.re("p (a b) -> p a b", b=MC)[:, g * HPG:(g + 1) * HPG, :])
        ld(selF, selF_d)
        ld(selV, selV_d)
        for (dst, ti) in ((KsT, 2), (KwT, 3)):
            d4 = dst.re("p (i r q) -> p i r q", r=CPB, q=128)
            for r in range(CPB):
                ld(d4[:, :, r, :], kT_all_v[ti * G + g][:, r, :].re("p (i q) -> p i q", q=128))
        for (dst, tsel) in ((Vs, 0), (Vw, 1)):
            cp(dst[:, :, 128:130], ones_f[:, 0:2 * NQ].re("p (a b) -> p a b", b=2))
            d4 = dst.re("p (i r) c -> p i r c", r=CPB)
            for r in range(CPB):
                for a_ in range(NVC):
                    ld(d4[:, a_ * NBV:(a_ + 1) * NBV, r, 0:128],
                       v_all_v[a_][:, r, :, (tsel * G + g) * 128:(tsel * G + g + 1) * 128])
        sc = A32.alloc([512], "sc")
        ex = A32.alloc([512], "ex")
        imp = A32.alloc([4 * 8 * NB + 8], "imp")
        isel = A32.alloc([8 * NB], "isel")
        score = A32.alloc([8 * NB], "score")
        scr2 = A32.alloc([8 * NB], "scr2")
        mx8 = A32.alloc([8], "mx8")
        msk = A32.alloc([8 * NB], "msk")
        mE = [A32.alloc([2, 64], "mE%d" % i_) for i_ in range(2)]
        MT = A16.alloc([NQ, 128], "MT")
        eT = A16.alloc([NCH, 128], "eT")
        pT = [A16.alloc([HB, 128], "pT%d" % i) for i in range(2)]
        small = A32.alloc([16], "small")
        ocmp = A32.alloc([HPG, 129], "ocmp")
        oacc = A32.alloc([128], "oacc")
        wcol = A32.alloc([8], "wcol")
        onT_st = A16.alloc([HPG, 128], "onT_st")
        for i in range(NB):
            Ni = min(32 * i + 31, NCMP)
            Ji = min(8 * i + 8, NSEL)
            m0 = 32 * (NB - 1 - i)
            f0 = 8 * (NB - 1 - i)
            nch_i = (Ni + 127) // 128
            memset(imp, 0.0)
            QT = QTb[i % 2]
            ld(QT, qT_v[:, g * HPG:(g + 1) * HPG, i * 128:(i + 1) * 128])
            ckpt('a0')
            for h in range(HPG):
                ps = banks[h % 2]
                mm(ps[:, 0:Ni], QT[:, h, :], KcT[:, 0:Ni], True, True)
                stt(sc[:, 0:Ni], ps[:, 0:Ni], scale, cmpb[:, h, m0:m0 + Ni], ALU.mult, ALU.add)
                red(small[:, 0:1], sc[:, 0:Ni], ALU.max)
                ts(small[:, 1:2], small[:, 0:1], -1.0, 20000.0, ALU.mult, ALU.min)
                act(ex[:, 0:Ni], sc[:, 0:Ni], AF.Exp, bias=small[:, 1:2])
                red(small[:, 2:3], ex[:, 0:Ni], ALU.add)
                ts(small[:, 3:4], small[:, 2:3], TINY, None, ALU.max)
                P.op("dve", lambda e, small=small: e.reciprocal(out=small.ap[:, 4:5], in_=small.ap[:, 3:4]),
                     reads=[small.b], writes=[small.b])
                stt(imp[:, 1:1 + Ni], ex[:, 0:Ni], small[:, 4:5], imp[:, 1:1 + Ni], ALU.mult, ALU.add)
                ckpt('a0c')
                for ch in range(nch_i):
                    n0 = ch * 128
                    nn = min(128, Ni - n0)
                    pt = banks[2 + ch % 2]
                    tr(pt[0:nn, 0:128], ex[:, n0:n0 + nn], ident)
                    cp(eT[0:nn, ch, :], pt[0:nn, 0:128])
                ckpt('a0d')
                po = banks[4]
                for ch in range(nch_i):
                    n0 = ch * 128
                    nn = min(128, Ni - n0)
                    mm(po[:, 0:129], eT[0:nn, ch, :], Vc[0:nn, ch, 0:129], ch == 0, ch == nch_i - 1)
                cp(ocmp[:, h, :], po[:, 0:129])
            ckpt('a1')
            impv = imp[:, 0:4 * Ji].re("p (j f) -> p j f", f=4)
            red(isel[:, 0:Ji], impv, ALU.add)
            tt(isel[:, 0:Ji], isel[:, 0:Ji], imp[:, 4:4 + 4 * Ji].re("p (j f) -> p j f", f=4)[:, :, 0], ALU.add)
            tt(score[:, 0:Ji], isel[:, 0:Ji], selF[:, f0:f0 + Ji], ALU.add)
            ts(score[:, 0:1], score[:, 0:1], 1.0e4, None, ALU.add)
            if Ji > TOPN:
                cur = score
                for rnd in range(TOPN // 8):
                    P.op("dve", lambda e, cur=cur, Ji=Ji, mx8=mx8: e.max(out=mx8.ap, in_=cur.ap[:, 0:Ji]), reads=[cur.b], writes=[mx8.b])
                    if rnd < TOPN // 8 - 1:
                        P.op("dve", lambda e, cur=cur, Ji=Ji, mx8=mx8, scr2=scr2: e.match_replace(out=scr2.ap[:, 0:Ji], in_to_replace=mx8.ap,
                                                                      in_values=cur.ap[:, 0:Ji], imm_value=-1.0e9),
                             reads=[cur.b, mx8.b], writes=[scr2.b])
                        cur = scr2
                stt(msk[:, 0:Ji], score[:, 0:Ji], mx8[:, 7:8], selV[:, f0:f0 + Ji], ALU.is_ge, ALU.mult)
            else:
                cp(msk[:, 0:Ji], selV[:, f0:f0 + Ji])
            ckpt('a1b')
            ntile = min(4 * i + 4, NQ)
            for j2 in range(ntile):
                pm = banks[2 + j2 % 2]
                me = mE[j2 % 2]
                P.op("dve", lambda e, me=me, j2=j2, msk=msk: e.tensor_copy(
                    out=me.ap, in_=msk.ap[:, 2 * j2:2 * j2 + 2].unsqueeze(2).to_broadcast([128, 2, 64])),
                    reads=[msk.b], writes=[me.b])
                tr(pm[:, 0:128], me.re("p a b -> p (a b)"), ident)
                if j2 >= 4 * i:
                    tt(MT[:, j2, :], pm[:, 0:128], camask[:, j2 - 4 * i, :], ALU.mult)
                else:
                    cp(MT[:, j2, :], pm[:, 0:128])
            ckpt('a2')
            for hb in range(HPG // HB):
                for br in range(2):
                    if br == 0:
                        tiles = list(range(ntile))
                        KT, VV = KsT, Vs
                    else:
                        tiles = [j for j in range(4 * i - 4, 4 * i + 4) if 0 <= j < NQ]
                        KT, VV = KwT, Vw
                    pos_ = [banks[4 + hh] for hh in range(HB)]
                    for n_, j2 in enumerate(tiles):
                        pS = banks[n_ % 2]
                        u = 4 * i - j2 + 3
                        mm(pS[:, 0:HB * 128].re("p (h q) -> p h q", q=128), KT[:, j2 * 128:(j2 + 1) * 128],
                           QT[:, hb * HB:(hb + 1) * HB, :], True, True)
                        pt_ = pT[n_ % 2]
                        for hh in range(HB):
                            hglob = g * HPG + hb * HB + hh
                            act(pt_[:, hh, :], pS[:, hh * 128:(hh + 1) * 128], AF.Exp,
                                bias=alibi[:, hglob, u:u + 1], scale=scale)
                        if br == 0:
                            mk = MT[:, j2, :]
                        else:
                            mk = wmask[:, j2 - (4 * i - 4), :]
                        for hh in range(HB):
                            tt(pt_[:, hh, :], pt_[:, hh, :], mk, ALU.mult, eng="pool" if hh % 2 else "dve")
                        for hh in range(HB):
                            mm(pos_[hh][:, 0:129], pt_[:, hh, :], VV[:, j2, 0:129], n_ == 0, n_ == len(tiles) - 1)
                    for hh in range(HB):
                        h = hb * HB + hh
                        hglob = g * HPG + h
                        po = pos_[hh]
                        ts(wcol[:, 0:1], po[:, 128:129], TINY, None, ALU.max)
                        P.op("dve", lambda e, wcol=wcol: e.reciprocal(out=wcol.ap[:, 1:2], in_=wcol.ap[:, 0:1]),
                             reads=[wcol.b], writes=[wcol.b])
                        tt(wcol[:, 2:3], wcol[:, 1:2], gsig[:, i, hglob * 3 + 1 + br:hglob * 3 + 2 + br], ALU.mult)
                        if br == 0:
                            ts(wcol[:, 3:4], ocmp[:, h, 128:129], TINY, None, ALU.max)
                            P.op("dve", lambda e, wcol=wcol: e.reciprocal(out=wcol.ap[:, 4:5], in_=wcol.ap[:, 3:4]),
                                 reads=[wcol.b], writes=[wcol.b])
                            tt(wcol[:, 5:6], wcol[:, 4:5], gsig[:, i, hglob * 3:hglob * 3 + 1], ALU.mult)
                            ts(ocmp[:, h, 0:128], ocmp[:, h, 0:128], wcol[:, 5:6], None, ALU.mult)
                            stt(ocmp[:, h, 0:128], po[:, 0:128], wcol[:, 2:3], ocmp[:, h, 0:128], ALU.mult, ALU.add)
                        else:
                            stt(oacc, po[:, 0:128], wcol[:, 2:3], ocmp[:, h, 0:128], ALU.mult, ALU.add)
                            ptr = banks[2 + hh % 2]
                            tr(ptr[:, 0:128], oacc, ident)
                            cp(onT_st[:, h, :], ptr[:, 0:128])
            ckpt('a3')
            ld(onT_v[:, g * HPG:(g + 1) * HPG, i * 128:(i + 1) * 128], onT_st)

    ckpt('pC')
    w_nsa_v = w_nsa_f.re("(k p) n -> p k n", p=128)
    w_cp_v = w_cp_f.re("(k p) n -> p k n", p=128)
    w_out_v = w_out_f.re("(k p) n -> p k n", p=128)
    x1T_send_v = x1T_send.re("(k p) n -> p k n", p=128)
    convw_v = convw_d.re("(k p) t -> p k t", p=128)
    for tg in range(NTG):
        new_phase()
        NW = BPG * BW
        x = A16.alloc([DK, NW], "xgD")
        ldc(x, xT_v[:, :, tg * NW:(tg + 1) * NW])
        x3 = x.re("p k (b w) -> p k b w", w=BW)
        wtD = [A16.alloc([DK, 128], "wtD%d" % i_) for i_ in range(2)]
        mT = A16.alloc([DK, TG], "mT")
        mark = AR.pos
        convw = A32.alloc([CK, 31], "convw")
        convb = A32.alloc([CK], "convb")
        clng = A32.alloc([CK], "clng")
        clnb = A32.alloc([CK], "clnb")
        ld(convw, convw_v)
        ld(convb, convb_d)
        ld(clng, clng_d)
        ld(clnb, clnb_d)
        hglu = A32.alloc([NW], "hglu")
        sg = A32.alloc([NW], "sg")
        hc = A32.alloc([CK, TG], "hc")
        hsq = A32.alloc([TG], "hsq")
        mean = A32.alloc([TG], "mean")
        rstd = A32.alloc([TG], "rstd")
        sT = A16.alloc([CK, TG], "sT")
        p_sum = banks[6]
        p_sq = banks[7]
        for ck in range(CK):
            wa = wtD[0]
            wb = wtD[1]
            ldc(wa[:, :, 0:128], w_in_v[:, :, off["glu"] + ck * 128:off["glu"] + (ck + 1) * 128])
            ldc(wb[:, :, 0:128], w_in_v[:, :, off["glu"] + CH + ck * 128:off["glu"] + CH + (ck + 1) * 128])
            for b0 in range(0, BPG, 3):
                nb_ = min(3, BPG - b0)
                pa = banks[0]
                pbk = banks[1]
                oa = pa[:, 0:nb_ * BW].re("p (b w) -> p b w", w=BW)
                ob = pbk[:, 0:nb_ * BW].re("p (b w) -> p b w", w=BW)
                for k in range(DK):
                    mm(oa, wa[:, k, 0:128], x3[:, k, b0:b0 + nb_, :], k == 0, k == DK - 1)
                for k in range(DK):
                    mm(ob, wb[:, k, 0:128], x3[:, k, b0:b0 + nb_, :], k == 0, k == DK - 1)
                act(sg[:, b0 * BW:(b0 + nb_) * BW], pbk[:, 0:nb_ * BW], AF.Sigmoid)
                tt(hglu[:, b0 * BW:(b0 + nb_) * BW], pa[:, 0:nb_ * BW], sg[:, b0 * BW:(b0 + nb_) * BW], ALU.mult)
            h3 = hglu.re("p (b w) -> p b w", w=BW)
            hcv = hc[:, ck, :].re("p (b q) -> p b q", q=128)
            ts(hcv, h3[:, :, 0:128], convw[:, ck, 0:1], convb[:, ck:ck + 1], ALU.mult, ALU.add)
            for tap in range(1, 31):
                stt(hcv, h3[:, :, tap:tap + 128], convw[:, ck, tap:tap + 1], hcv, ALU.mult, ALU.add)
            act(hsq, hc[:, ck, :], AF.Square)
            mm(p_sum[:, 0:TG], ones_f, hc[:, ck, :], ck == 0, ck == CK - 1)
            mm(p_sq[:, 0:TG], ones_f, hsq, ck == 0, ck == CK - 1)
        ckpt('d1')
        ts(mean, p_sum[:, 0:TG], 1.0 / CH, None, ALU.mult)
        tt(rstd, mean, mean, ALU.mult)
        stt(rstd, p_sq[:, 0:TG], 1.0 / CH, rstd, ALU.mult, ALU.subtract)
        ts(rstd, rstd, 0.0, None, ALU.max)
        act(rstd, rstd, AF.Sqrt, bias=eps_t)
        P.op("dve", lambda e, rstd=rstd: e.reciprocal(out=rstd.ap, in_=rstd.ap), reads=[rstd.b], writes=[rstd.b])
        for ck in range(CK):
            tt(hc[:, ck, :], hc[:, ck, :], mean, ALU.subtract)
            tt(hc[:, ck, :], hc[:, ck, :], rstd, ALU.mult)
            act(sT[:, ck, :], hc[:, ck, :], AF.Silu, bias=clnb[:, ck:ck + 1], scale=clng[:, ck:ck + 1])
        ckpt('d2')
        onT = A16.alloc([H, TG], "onT")
        ld(onT, onT_v[:, :, tg * TG:(tg + 1) * TG])
        wn = [A16.alloc([H, 128], "wn%d" % i_) for i_ in range(2)]
        wc = [A16.alloc([CK, 128], "wc%d" % i_) for i_ in range(2)]
        ga = A32.alloc([TG], "ga")
        gb = A32.alloc([TG], "gb")
        for dm in range(DK):
            wa = wtD[0]
            wb = wtD[1]
            ldc(wa[:, :, 0:128], w_in_v[:, :, off["mrg"] + dm * 128:off["mrg"] + (dm + 1) * 128])
            ldc(wb[:, :, 0:128], w_in_v[:, :, off["mrg"] + D + dm * 128:off["mrg"] + D + (dm + 1) * 128])
            wnn = wn[dm % 2]
            wcc = wc[dm % 2]
            ldc(wnn, w_nsa_v[:, :, dm * 128:(dm + 1) * 128])
            ldc(wcc, w_cp_v[:, :, dm * 128:(dm + 1) * 128])
            pga, pgb, pya, pyb = banks[0], banks[1], banks[2], banks[3]
            oga = pga[:, 0:TG].re("p (b q) -> p b q", q=128)
            ogb = pgb[:, 0:TG].re("p (b q) -> p b q", q=128)
            for k in range(DK):
                mm(oga, wa[:, k, 0:128], x3[:, k, :, HALO:BW], k == 0, k == DK - 1)
            for k in range(DK):
                mm(ogb, wb[:, k, 0:128], x3[:, k, :, HALO:BW], k == 0, k == DK - 1)
            for k in range(H):
                mm(pya[:, 0:TG], wnn[:, k, :], onT[:, k, :], k == 0, k == H - 1)
            for k in range(CK):
                mm(pyb[:, 0:TG], wcc[:, k, :], sT[:, k, :], k == 0, k == CK - 1)
            act(ga, pga[:, 0:TG], AF.Sigmoid)
            act(gb, pgb[:, 0:TG], AF.Sigmoid)
            tt(ga, ga, pya[:, 0:TG], ALU.mult)
            tt(gb, gb, pyb[:, 0:TG], ALU.mult)
            tt(mT[:, dm, :], ga, gb, ALU.add)
        ckpt('d3')
        P.barrier()
        AR.pos = mark
        wo = [A16.alloc([DK, 512], "wo%d" % i_) for i_ in range(2)]
        xin = A32.alloc([D], "xin")
        stats = A32.alloc([8, 6], "stats")
        mv = A32.alloc([4], "mv")
        lg = A32.alloc([D], "lg")
        lb = A32.alloc([D], "lb")
        ldb(lg, ln1g_d, D)
        ldb(lb, ln1b_d, D)
        NDC = (D + 511) // 512
        x1b = A32.alloc([D], "x1b")
        xTst = A16.alloc([DK, 128], "xTst")
        rws = A32.alloc([DK, E], "rws")
        ld(rws, rw_d.re("(k p) e -> p k e", p=128))
        rbs = A32.alloc([E], "rbs")
        ldb(rbs, rb_d, E)
        x1T32 = A32.alloc([DK, 128], "x1T32")
        lgt = A32.alloc([E], "lgt")
        lg2 = A32.alloc([E], "lg2")
        gts = A32.alloc([E], "gts")
        mxr = A32.alloc([8], "mxr")
        sm = A32.alloc([4], "sm")
        for b in range(BPG):
            row0 = (tg * BPG + b) * 128
            ld(xin, xtok_d[row0:row0 + 128, :])
            for dc in range(NDC):
                cw = min(512, D - dc * 512)
                w = wo[dc % 2]
                if b == 0 or NDC > 2:
                    ldc(w[:, :, 0:cw], w_out_v[:, :, dc * 512:dc * 512 + cw])
                pm = banks[4 + dc % 2]
                for k in range(DK):
                    mm(pm[:, 0:cw], mT[:, k, b * 128:(b + 1) * 128], w[:, k, 0:cw], k == 0, k == DK - 1)
                stt(xin[:, dc * 512:dc * 512 + cw], xin[:, dc * 512:dc * 512 + cw], ALPHA, pm[:, 0:cw], ALU.mult, ALU.add)
                P.op("dve", lambda e, dc=dc, cw=cw, stats=stats, xin=xin: e.bn_stats(out=stats.ap[:, dc, :], in_=xin.ap[:, dc * 512:dc * 512 + cw]),
                     reads=[xin.b], writes=[stats.b])
            P.op("dve", lambda e, mv=mv, stats=stats, NDC=NDC: e.bn_aggr(out=mv.ap[:, 0:2], in_=stats.ap.rearrange("p a b -> p (a b)")[:, 0:NDC * 6]), reads=[stats.b], writes=[mv.b])
            act(mv[:, 2:3], mv[:, 1:2], AF.Sqrt, bias=eps_t)
            P.op("dve", lambda e, mv=mv: e.reciprocal(out=mv.ap[:, 3:4], in_=mv.ap[:, 2:3]), reads=[mv.b], writes=[mv.b])
            ts(x1b, xin, mv[:, 0:1], mv[:, 3:4], ALU.subtract, ALU.mult)
            tt(x1b, x1b, lg, ALU.mult)
            tt(x1b, x1b, lb, ALU.add)
            ckpt('d4')
            ld(x1_s[row0:row0 + 128, :], x1b)
            for k in range(DK):
                ptr = banks[k % 2]
                tr(ptr[:, 0:128], x1b[:, k * 128:(k + 1) * 128], ident)
                cp(x1T32[:, k, :], ptr[:, 0:128])
                act(xTst[:, k, :], x1T32[:, k, :], AF.Copy)
            ld(x1T_send_v[:, :, row0:row0 + 128], xTst)
            ckpt('d5')
            pr = banks[2]
            for k in range(DK):
                mm(pr[:, 0:E], x1T32[:, k, :], rws[:, k, :], k == 0, k == DK - 1)
            tt(lgt, pr[:, 0:E], rbs, ALU.add)
            P.op("dve", lambda e, mxr=mxr, lgt=lgt: e.max(out=mxr.ap, in_=lgt.ap), reads=[lgt.b], writes=[mxr.b])
            ts(lg2, lgt, mxr[:, 3:4], None, ALU.is_ge)
            ts(gts, lgt, mxr[:, 0:1], None, ALU.subtract)
            act(gts, gts, AF.Exp)
            tt(gts, gts, lg2, ALU.mult)
            red(sm[:, 0:1], gts, ALU.add)
            P.op("dve", lambda e, sm=sm: e.reciprocal(out=sm.ap[:, 1:2], in_=sm.ap[:, 0:1]), reads=[sm.b], writes=[sm.b])
            ts(gts, gts, sm[:, 1:2], None, ALU.mult)
            ld(gate_send[row0:row0 + 128, :], gts)

    ckpt('pD')
    for a_ in range(DK):
        ag8(x1T_send[a_ * 128:(a_ + 1) * 128, :], x1T_all[a_])
    ag8(gate_send, gate_all)
    x1T_all_v = [t_.re("(r p) n -> p r n", p=128) for t_ in x1T_all]
    hT_v = hT_s.re("(k p) n -> p k n", p=128)
    TT = min(512, NT)
    NTT = NT // TT
    FH = max(1, FK // 8)
    FC = FK // FH
    for el in range(EPC):
        for fh in range(FH):
            new_phase()
            wgs = A16.alloc([DK, FC * 128], "wgs")
            wus = A16.alloc([DK, FC * 128], "wus")
            ldc(wgs, wg_d[el].re("(k p) f -> p k f", p=128)[:, :, fh * FC * 128:(fh + 1) * FC * 128])
            ldc(wus, wu_d[el].re("(k p) f -> p k f", p=128)[:, :, fh * FC * 128:(fh + 1) * FC * 128])
            bgs = A32.alloc([FC], "bgs")
            bus = A32.alloc([FC], "bus")
            ld(bgs, bg_d[el][:, fh * FC:(fh + 1) * FC])
            ld(bus, bu_d[el][:, fh * FC:(fh + 1) * FC])
            xt = [A16.alloc([DK, TT], "xt%d" % i_) for i_ in range(2)]
            hst = [A16.alloc([TT], "hst%d" % i_) for i_ in range(2)]
            gg = A32.alloc([TT], "gg")
            uu = A32.alloc([TT], "uu")
            sgm = A32.alloc([TT], "sgm")
            xtiles = [(r, t) for r in range(NCORES) for t in range(NTT)]

            def load_x(n_):
                r, t = xtiles[n_]
                for k in range(DK):
                    ld(xt[n_ % 2][:, k, :], x1T_all_v[k][:, r, t * TT:(t + 1) * TT])

            load_x(0)
            for n_, (r, t) in enumerate(xtiles):
                if True:
                    if n_ + 1 < len(xtiles):
                        load_x(n_ + 1)
                    xx = xt[n_ % 2]
                    col0 = r * NT + t * TT
                    for fo in range(FC):
                        pg = banks[(2 * fo) % 4]
                        pu = banks[(2 * fo + 1) % 4]
                        for k in range(DK):
                            mm(pg[:, 0:TT], wgs[:, k, fo * 128:(fo + 1) * 128], xx[:, k, :], k == 0, k == DK - 1)
                        for k in range(DK):
                            mm(pu[:, 0:TT], wus[:, k, fo * 128:(fo + 1) * 128], xx[:, k, :], k == 0, k == DK - 1)
                        ts(gg, pg[:, 0:TT], bgs[:, fo:fo + 1], 7.0, ALU.add, ALU.min)
                        ts(uu, pu[:, 0:TT], bus[:, fo:fo + 1], 7.0, ALU.add, ALU.min)
                        ts(uu, uu, -7.0, 1.0, ALU.max, ALU.add)
                        act(sgm, gg, AF.Sigmoid, scale=1.702)
                        tt(gg, gg, sgm, ALU.mult, eng="pool")
                        hs = hst[fo % 2]
                        tt(hs, gg, uu, ALU.mult)
                        ld(hT_v[:, fh * FC + fo, col0:col0 + TT], hs)
        new_phase()
        wds = A16.alloc([FK, D], "wds")
        ldc(wds, wd_d[el].re("(k p) n -> p k n", p=128))
        bdr = A32.alloc([D], "bdr")
        ldb(bdr, bd_d[el], D)
        selm = A32.alloc([EPC, E], "selm")
        ldb(selm.re("p a b -> p (a b)"), selm_d, EPC * E)
        ht = [A16.alloc([FK, 128], "ht%d" % i_) for i_ in range(2)]
        gcol = [A32.alloc([E], "gcol%d" % i_) for i_ in range(2)]
        gtmp = A32.alloc([E], "gtmp")
        gsel = A32.alloc([2], "gsel")
        yo = [A32.alloc([D], "yo%d" % i_) for i_ in range(2)]
        yp = [A32.alloc([D], "yp%d" % i_) for i_ in range(2)]
        NDC2 = (D + 511) // 512
        def load_b(tc):
            ld(ht[tc % 2], hT_v[:, :, tc * 128:(tc + 1) * 128])
            ld(gcol[tc % 2], gate_all[tc * 128:(tc + 1) * 128, :])
            if el > 0:
                ld(yp[tc % 2], part_s[tc * 128:(tc + 1) * 128, :])

        load_b(0)
        for tc in range(TALL // 128):
            if tc + 1 < TALL // 128:
                load_b(tc + 1)
            hh_ = ht[tc % 2]
            gc = gcol[tc % 2]
            tt(gtmp, gc, selm[:, el, :], ALU.mult)
            red(gsel[:, 0:1], gtmp, ALU.add)
            y = yo[tc % 2]
            if el > 0:
                ypv = yp[tc % 2]
            for dc in range(NDC2):
                cw = min(512, D - dc * 512)
                pm = banks[dc % 4]
                for k in range(FK):
                    mm(pm[:, 0:cw], hh_[:, k, :], wds[:, k, dc * 512:dc * 512 + cw], k == 0, k == FK - 1)
                tt(y[:, dc * 512:dc * 512 + cw], pm[:, 0:cw], bdr[:, dc * 512:dc * 512 + cw], ALU.add)
            if el > 0:
                stt(y, y, gsel[:, 0:1], ypv, ALU.mult, ALU.add)
            else:
                ts(y, y, gsel[:, 0:1], None, ALU.mult)
            ld(part_s[tc * 128:(tc + 1) * 128, :], y)

    ckpt('pE')
    rs8(part_s, f_s)

    new_phase()
    lg = A32.alloc([D], "lg2g")
    lb = A32.alloc([D], "lg2b")
    ldb(lg, ln2g_d, D)
    ldb(lb, ln2b_d, D)
    xa = [A32.alloc([D], "xa%d" % i_) for i_ in range(2)]
    fa = [A32.alloc([D], "fa%d" % i_) for i_ in range(2)]
    stats = A32.alloc([8, 6], "stats2")
    mv = A32.alloc([4], "mv2")
    NDC = (D + 511) // 512
    for b in range(NB):
        xv = xa[b % 2]
        fv = fa[b % 2]
        ld(xv, x1_s[b * 128:(b + 1) * 128, :])
        ld(fv, f_s[b * 128:(b + 1) * 128, :])
        stt(xv, xv, ALPHA, fv, ALU.mult, ALU.add)
        for dc in range(NDC):
            cw = min(512, D - dc * 512)
            P.op("dve", lambda e, dc=dc, cw=cw, xv=xv, stats=stats: e.bn_stats(out=stats.ap[:, dc, :], in_=xv.ap[:, dc * 512:dc * 512 + cw]),
                 reads=[xv.b], writes=[stats.b])
        P.op("dve", lambda e, mv=mv, stats=stats, NDC=NDC: e.bn_aggr(out=mv.ap[:, 0:2], in_=stats.ap.rearrange("p a b -> p (a b)")[:, 0:NDC * 6]), reads=[stats.b], writes=[mv.b])
        act(mv[:, 2:3], mv[:, 1:2], AF.Sqrt, bias=eps_t)
        P.op("dve", lambda e, mv=mv: e.reciprocal(out=mv.ap[:, 3:4], in_=mv.ap[:, 2:3]), reads=[mv.b], writes=[mv.b])
        ts(fv, xv, mv[:, 0:1], mv[:, 3:4], ALU.subtract, ALU.mult)
        tt(fv, fv, lg, ALU.mult)
        tt(fv, fv, lb, ALU.add)
        ld(out_d[b * 128:(b + 1) * 128, :], fv)
    P.barrier()
    P.finish()
    return nc, c


def _tables(c, r):
    NQ, NB, H = c["NQ"], c["NB"], c["H"]
    slopes = np.exp2(-8.0 * np.arange(1, H + 1, dtype=np.float64) / H)
    k = np.arange(128, dtype=np.float64)
    u = np.arange(NQ + 3)
    delta = u - 3 + r
    rel = (-128.0 * delta[None, :] + k[:, None] - 64.0)
    al = slopes[None, :, None] * rel[:, None, :]
    al = np.where(delta[None, None, :] >= 0, al, NEG)
    alibi = al.reshape(128, H * (NQ + 3)).astype(np.float32)
    ki = np.arange(128)[:, None]
    qi = np.arange(128)[None, :]
    wm = np.zeros((128, 8, 128), np.float32)
    for dj in range(8):
        d = r + 4 - dj
        if d == 0:
            wm[:, dj, :] = (ki <= qi)
        elif 1 <= d <= 3:
            wm[:, dj, :] = 1.0
        elif d == 4:
            wm[:, dj, :] = (ki > qi)
    ca = np.zeros((128, 4, 128), np.float32)
    for dj in range(4):
        d = r - dj
        if d > 0:
            ca[:, dj, :] = 1.0
        elif d == 0:
            ca[:, dj, :] = (ki <= qi)
    MC = 32 * NB
    m = np.arange(MC)
    npr = m - 32 * (NB - 1) - 8 * r
    dist = np.arange(128)[:, None] - 16.0 * npr[None, :] - 31.0
    cb = -slopes[None, :, None] * dist[:, None, :]
    cb = np.where(dist[:, None, :] >= 0, cb, NEG)
    cmpb = cb.reshape(128, H * MC).astype(np.float32)
    mm_ = np.arange(8 * NB)
    jp = mm_ - 8 * (NB - 1) - 2 * r
    q = np.arange(128)[:, None]
    curp = (q >= 64).astype(np.int64)
    valid = (64 * jp[None, :] <= q)
    forced = (jp[None, :] == curp) | (jp[None, :] == curp - 1)
    selF = np.where(valid, np.where(forced, 1.0e4, 0.0), -1.0e9).astype(np.float32)
    selV = valid.astype(np.float32)
    return dict(alibi_tab=alibi, wmask=wm.reshape(128, 8 * 128).astype(ml_dtypes.bfloat16),
                camask=ca.reshape(128, 4 * 128).astype(ml_dtypes.bfloat16), cmpb=cmpb, selF=selF, selV=selV)


def make_in_maps(c, inp):
    S, D, H, E, CH = c["S"], c["D"], c["H"], c["E"], c["CH"]
    NB, NT, EPC = c["NB"], c["NT"], c["EPC"]
    f32 = lambda a: np.ascontiguousarray(a, dtype=np.float32)
    x = inp["x"]
    shared = dict(
        cmp_peT=f32(inp["cmp_pe"][0].transpose(0, 2, 1)),
        cmp_w1=f32(inp["cmp_w1"][0]),
        cmp_b1=f32(inp["cmp_b1"][0].reshape(2, 128, 1)),
        cmp_w2=f32(inp["cmp_w2"][0]),
        cmp_b2=f32(inp["cmp_b2"][0].reshape(2, 128, 1)),
        cmp_b2r=f32(inp["cmp_b2"][0].reshape(2, 1, 128)),
        conv_wT=f32(inp["conv_w"][0].T),
        conv_b=f32(inp["conv_b"][0].reshape(CH // 128, 128).T),
        conv_ln_g=f32(inp["conv_ln_g"][0].reshape(CH // 128, 128).T),
        conv_ln_b=f32(inp["conv_ln_b"][0].reshape(CH // 128, 128).T),
        ln1_g=f32(inp["ln1_g"][0].reshape(1, D)), ln1_b=f32(inp["ln1_b"][0].reshape(1, D)),
        ln2_g=f32(inp["ln2_g"][0].reshape(1, D)), ln2_b=f32(inp["ln2_b"][0].reshape(1, D)),
        router_w=f32(inp["router_w"][0]), router_b=f32(inp["router_b"][0].reshape(1, E)),
        ident=np.eye(128, dtype=np.float32),
    )
    maps = []
    for k in range(NCORES):
        b, r = k // CPB, k % CPB
        xT = np.zeros((D, NB * BW), np.float32)
        xtok = np.zeros((NT, D), np.float32)
        for i in range(NB):
            cblk = CPB * i + r
            t0 = 128 * cblk - HALO
            lo = max(t0, 0)
            xT[:, i * BW + (lo - t0):(i + 1) * BW] = x[b, lo:128 * cblk + 128].T
            xtok[i * 128:(i + 1) * 128] = x[b, 128 * cblk:128 * cblk + 128]
        m = dict(shared)
        m.update(_tables(c, r))
        selm = np.zeros((EPC, E), np.float32)
        for el in range(EPC):
            selm[el, k * EPC + el] = 1.0
        sh = lambda w: f32(w[k * (w.shape[0] // 8):(k + 1) * (w.shape[0] // 8)])
        m.update(
            xT=xT, xtok=xtok,
            w_in=f32(inp["w_in"][0]), w_nsa=f32(inp["w_nsa_proj"][0]),
            w_cp=f32(inp["w_conv_proj"][0]), w_out=f32(inp["w_out"][0]),
            w_gate=f32(inp["w_gate"][0][k * EPC:(k + 1) * EPC]),
            w_up=f32(inp["w_up"][0][k * EPC:(k + 1) * EPC]),
            w_down=f32(inp["w_down"][0][k * EPC:(k + 1) * EPC]),
            b_gate=f32(inp["b_gate"][0][k * EPC:(k + 1) * EPC].reshape(EPC, -1, 128).transpose(0, 2, 1)),
            b_up=f32(inp["b_up"][0][k * EPC:(k + 1) * EPC].reshape(EPC, -1, 128).transpose(0, 2, 1)),
            b_down=f32(inp["b_down"][0][k * EPC:(k + 1) * EPC].reshape(EPC, 1, D)),
            selm=selm.reshape(1, EPC * E),
        )
        maps.append(m)
    return maps


def run_cfg(cfg, inp, trace=False, stop_after=None):
    nc, c = build_program(cfg, stop_after)
    maps = make_in_maps(c, inp)
    res = run_bass_kernel_spmd(nc, maps, core_ids=list(range(NCORES)))
    S, D, NB = c["S"], c["D"], c["NB"]
    out = np.zeros((2, S, D), np.float32)
    for k in range(NCORES):
        b, r = k // CPB, k % CPB
        o = res.results[k]["out"]
        for i in range(NB):
            cblk = CPB * i + r
            out[b, 128 * cblk:128 * cblk + 128] = o[i * 128:(i + 1) * 128]
    return out


def kernel(**inputs):
    inp = {k: np.asarray(v) for k, v in inputs.items()}
    return run_cfg(CFG_FULL, inp)
```

```python
import contextlib
import math
import numpy as np
import ml_dtypes
import concourse.bass as bass
import concourse.mybir as mybir
from concourse.bass_utils import run_bass_kernel_spmd

F32 = mybir.dt.float32
BF16 = mybir.dt.bfloat16
AF = mybir.ActivationFunctionType
ALU = mybir.AluOpType
AX = mybir.AxisListType

NCORES = 8
CPB = 4
G = 2
DH = 128
HALO = 30
BW = 128 + HALO
NEG = -30000.0
TINY = 1e-30

CFG_FULL = dict(S=8192, D=2048, H=16, TOPN=16, E=32, DFF=2048, CH=1024)


class Buf:
    __slots__ = ("name", "w", "r")

    def __init__(self, name=""):
        self.name = name
        self.w = {}
        self.r = {}


class V:
    __slots__ = ("ap", "b")

    def __init__(self, ap, b):
        self.ap = ap
        self.b = b

    def __getitem__(self, idx):
        return V(self.ap[idx], self.b)

    def re(self, pat, **kw):
        return V(self.ap.rearrange(pat, **kw), self.b)


class Prog:
    ENGS = ("pe", "act", "dve", "pool", "sp")

    def __init__(self, nc, n_ring=8):
        self.nc = nc
        self.es = contextlib.ExitStack()
        self.lists = {e: [] for e in self.ENGS}
        self.sems = {}
        self.cnt = {}
        for e in ("pe", "act", "dve", "pool"):
            self.sems[e] = self.es.enter_context(nc.semaphore("s_" + e))
            self.cnt[e] = 0
        self.ring = {}
        self.ring_pos = {}
        for q in ("sp", "pool"):
            self.ring[q] = []
            for i in range(n_ring):
                k = "d_%s%d" % (q, i)
                self.sems[k] = self.es.enter_context(nc.semaphore(k))
                self.cnt[k] = 0
                self.ring[q].append(k)
            self.ring_pos[q] = 0
        self.sems["cc"] = self.es.enter_context(nc.semaphore("s_cc"))
        self.cnt["cc"] = 0
        self.waited = {e: {} for e in self.ENGS}
        self.n_inst = 0

    def _need(self, eng, k, v, waits):
        if eng == "pe" and k == "pe":
            return
        if self.waited[eng].get(k, 0) >= v:
            return
        if waits.get(k, 0) < v:
            waits[k] = v

    def _deps(self, eng, reads, writes):
        waits = {}
        for b in reads:
            for k, v in b.w.items():
                self._need(eng, k, v, waits)
        for b in writes:
            for k, v in b.w.items():
                self._need(eng, k, v, waits)
            for k, v in b.r.items():
                self._need(eng, k, v, waits)
        for k, v in waits.items():
            self.waited[eng][k] = v
        return list(waits.items())

    def _commit(self, tok, reads, writes):
        k, v = tok
        for b in reads:
            if b.r.get(k, 0) < v:
                b.r[k] = v
        for b in writes:
            b.w[k] = v
            b.r = {}

    def op(self, eng, fn, reads=(), writes=()):
        waits = self._deps(eng, reads, writes)
        self.cnt[eng] += 1
        tok = (eng, self.cnt[eng])
        self.lists[eng].append(("op", fn, waits, eng, self.cnt[eng]))
        self._commit(tok, reads, writes)
        self.n_inst += 1

    def dma(self, q, fn, reads=(), writes=()):
        pos = self.ring_pos[q]
        self.ring_pos[q] = (pos + 1) % len(self.ring[q])
        k = self.ring[q][pos]
        waits = self._deps(q, reads, writes)
        prev = self.cnt[k]
        if prev > 0 and self.waited[q].get(k, 0) < prev:
            self.waited[q][k] = prev
            waits = waits + [(k, prev)]
        self.cnt[k] += 16
        tok = (k, self.cnt[k])
        self.lists[q].append(("dma", fn, waits, k, 16))
        self._commit(tok, reads, writes)
        self.n_inst += 1

    def collective(self, fn, reads=(), writes=()):
        waits = self._deps("pool", reads, writes)
        prev = self.cnt["cc"]
        if prev > 0 and self.waited["pool"].get("cc", 0) < prev:
            self.waited["pool"]["cc"] = prev
            waits = waits + [("cc", prev)]
        self.cnt["cc"] += 1
        tok = ("cc", self.cnt["cc"])
        self.lists["pool"].append(("dma", fn, waits, "cc", 1))
        self._commit(tok, reads, writes)

    def barrier(self):
        for eng in self.ENGS:
            waits = []
            for k, v in self.cnt.items():
                if v > 0 and self.waited[eng].get(k, 0) < v and not (eng == "pe" and k == "pe"):
                    self.waited[eng][k] = v
                    waits.append((k, v))
            self.lists[eng].append(("bar", None, waits, None, 0))

    def finish(self):
        nc = self.nc
        L = self.lists
        comp = ("pe", "act", "dve", "pool")
        W = {e: set() for e in comp}
        for eng in self.ENGS:
            for _, _, waits, _, _ in L[eng]:
                for k, v in waits:
                    if k in W:
                        W[k].add(v)
        rank = {e: {v: i + 1 for i, v in enumerate(sorted(W[e]))} for e in comp}
        self.n_incs = {e: len(W[e]) for e in comp}
        sems = self.sems

        def run(e, recs):
            for kind, fn, waits, key, n in recs:
                for k, v in waits:
                    e.wait_ge(sems[k], rank[k][v] if k in rank else v)
                if kind == "op":
                    r = fn(e)
                    if n in W[key]:
                        r.then_inc(sems[key], 1)
                elif kind == "dma":
                    fn(e).then_inc(sems[key], n)

        with nc.Block() as block:
            @block.tensor
            def _(e):
                run(e, L["pe"])

            @block.scalar
            def _(e):
                run(e, L["act"])

            @block.vector
            def _(e):
                run(e, L["dve"])

            @block.gpsimd
            def _(e):
                run(e, L["pool"])

            @block.sync
            def _(e):
                run(e, L["sp"])
        self.es.close()


class Arena:
    def __init__(self, P, name, ncols):
        self.t = P.es.enter_context(P.nc.sbuf_tensor(name, [128, ncols], F32))
        self.n = ncols
        self.pos = 0
        self.name = name

    def reset(self):
        self.pos = 0

    def alloc(self, shape, dtype, name=""):
        ne = int(np.prod(shape))
        n32 = ne if dtype == F32 else (ne + 1) // 2
        n32 = (n32 + 7) // 8 * 8
        assert self.pos + n32 <= self.n, "arena overflow: %s needs %d at %d/%d" % (name, n32, self.pos, self.n)
        ap = self.t[:, self.pos:self.pos + n32]
        self.pos += n32
        if dtype != F32:
            ap = ap.bitcast(dtype)
        ap = ap[:, 0:ne]
        if len(shape) == 2:
            ap = ap.rearrange("p (a b) -> p a b", b=shape[1])
        elif len(shape) == 3:
            ap = ap.rearrange("p (a b c) -> p a b c", b=shape[1], c=shape[2])
        return V(ap, Buf(name))


def derive(cfg):
    c = dict(cfg)
    S, D, H, E = c["S"], c["D"], c["H"], c["E"]
    c["NQ"] = S // 128
    c["NB"] = c["NQ"] // CPB
    c["NT"] = c["NB"] * 128
    c["HPG"] = H // G
    c["NCMP"] = (S - 32) // 16 + 1
    c["NSEL"] = S // 64
    c["DK"] = D // 128
    c["EPC"] = E // NCORES
    c["TALL"] = NCORES * c["NT"]
    off = {}
    o = 0
    for nm, w in (("q", H * DH), ("kc", G * DH), ("vc", G * DH), ("ks", G * DH), ("vs", G * DH),
                  ("kw", G * DH), ("vw", G * DH), ("gn", H * 3), ("glu", 2 * c["CH"]), ("mrg", 2 * D)):
        off[nm] = o
        o += w
    c["off"] = off
    c["IN_DIM"] = o
    return c


class _Stop(Exception):
    pass


def build_program(cfg, stop_after=None):
    try:
        return _build_program(cfg, stop_after)
    except _Stop as e:
        return e.args


def _build_program(cfg, stop_after=None):
    c = derive(cfg)
    S, D, H, E, DFF, CH, TOPN = c["S"], c["D"], c["H"], c["E"], c["DFF"], c["CH"], c["TOPN"]
    NQ, NB, NT, HPG, NCMP, NSEL, DK, EPC, TALL = (c["NQ"], c["NB"], c["NT"], c["HPG"], c["NCMP"],
                                                   c["NSEL"], c["DK"], c["EPC"], c["TALL"])
    off, IN_DIM = c["off"], c["IN_DIM"]
    CK = CH // 128
    FK = DFF // 128
    scale = 1.0 / math.sqrt(DH)
    ALPHA = 2.0 ** 0.25
    NTG = max(1, NB // 4)
    BPG = NB // NTG
    TG = BPG * 128
    assert TG <= 512

    nc = bass.Bass("TRN2", target_bir_lowering=False)
    P = Prog(nc)

    def din(name, shape, dt=F32):
        return V(nc.dram_tensor(name, list(shape), dt, kind="ExternalInput").ap(), Buf(name))

    def dtmp(name, shape, dt=F32):
        return V(nc.dram_tensor(name, list(shape), dt).ap(), Buf(name))

    xT_d = din("xT", [D, NB * BW])
    xtok_d = din("xtok", [NT, D])
    w_in_f = din("w_in", [D, IN_DIM])
    w_nsa_f = din("w_nsa", [H * DH, D])
    w_cp_f = din("w_cp", [CH, D])
    w_out_f = din("w_out", [D, D])
    peT_d = din("cmp_peT", [2, 128, 32])
    w1_d = din("cmp_w1", [2, 32 * 128, 128])
    b1_d = din("cmp_b1", [2, 128, 1])
    w2_d = din("cmp_w2", [2, 128, 128])
    b2_d = din("cmp_b2", [2, 128, 1])
    b2r_d = din("cmp_b2r", [2, 1, 128])
    convw_d = din("conv_wT", [CH, 31])
    convb_d = din("conv_b", [128, CH // 128])
    clng_d = din("conv_ln_g", [128, CH // 128])
    clnb_d = din("conv_ln_b", [128, CH // 128])
    ln1g_d = din("ln1_g", [1, D])
    ln1b_d = din("ln1_b", [1, D])
    ln2g_d = din("ln2_g", [1, D])
    ln2b_d = din("ln2_b", [1, D])
    rw_d = din("router_w", [D, E])
    rb_d = din("router_b", [1, E])
    wg_d = din("w_gate", [EPC, D, DFF])
    wu_d = din("w_up", [EPC, D, DFF])
    wd_d = din("w_down", [EPC, DFF, D])
    bg_d = din("b_gate", [EPC, 128, DFF // 128])
    bu_d = din("b_up", [EPC, 128, DFF // 128])
    bd_d = din("b_down", [EPC, 1, D])
    ident_d = din("ident", [128, 128])
    alibi_d = din("alibi_tab", [128, H * (NQ + 3)])
    wmask_d = din("wmask", [128, 8 * 128], BF16)
    camask_d = din("camask", [128, 4 * 128], BF16)
    MC = 32 * NB
    cmpb_d = din("cmpb", [128, H * MC])
    selF_d = din("selF", [128, 8 * NB])
    selV_d = din("selV", [128, 8 * NB])
    selm_d = din("selm", [1, EPC * E])
    out_d = V(nc.dram_tensor("out", [NT, D], F32, kind="ExternalOutput").ap(), Buf("out"))

    qT_s = dtmp("qT_s", [H * 128, NT], BF16)
    kT_send = dtmp("kT_send", [4 * G * 128, NT], BF16)
    kT_all = [dtmp("kT_all%d" % a, [CPB * 128, NT], BF16) for a in range(4 * G)]
    v_send = dtmp("v_send", [NT, 2 * G * 128], BF16)
    NVC = max(1, NT // 1024)
    NBV = NB // NVC
    v_all = [dtmp("v_all%d" % a, [CPB * NBV * 128, 2 * G * 128], BF16) for a in range(NVC)]
    onT_s = dtmp("onT_s", [H * 128, NT], BF16)
    x1_s = dtmp("x1_s", [NT, D])
    x1T_send = dtmp("x1T_send", [D, NT], BF16)
    x1T_all = [dtmp("x1T_all%d" % a, [NCORES * 128, NT], BF16) for a in range(DK)]
    gate_send = dtmp("gate_send", [NT, E])
    gate_all = dtmp("gate_all", [TALL, E])
    hT_s = dtmp("hT_s", [DFF, TALL], BF16)
    part_s = dtmp("part_s", [TALL, D])
    f_s = dtmp("f_s", [NT, D])

    AR = Arena(P, "arena", 39 * 1024)

    class _A:
        def __init__(self, dt):
            self.dt = dt

        def alloc(self, shape, name=""):
            return AR.alloc(shape, self.dt, name)

    A16 = _A(BF16)
    A32 = _A(F32)
    banks = [V(P.es.enter_context(nc.psum_tensor("pb%d" % i, [128, 512], F32))[:], Buf("pb%d" % i)) for i in range(8)]

    def ckpt(name):
        if stop_after == name:
            P.barrier()
            P.finish()
            raise _Stop(nc, c)

    def new_phase():
        P.barrier()
        AR.reset()

    def mm(out, lhsT, rhs, start, stop):
        P.op("pe", lambda e: e.matmul(out.ap, lhsT=lhsT.ap, rhs=rhs.ap, start=start, stop=stop),
             reads=[lhsT.b, rhs.b], writes=[out.b])

    def tr(out, in_, idn):
        P.op("pe", lambda e: e.transpose(out.ap, in_.ap, idn.ap), reads=[in_.b, idn.b], writes=[out.b])

    def act(out, in_, func, bias=None, scale=None, accum=None, eng="act"):
        kw = {}
        rd = [in_.b]
        wr = [out.b]
        if bias is not None:
            if isinstance(bias, V):
                kw["bias"] = bias.ap
                rd.append(bias.b)
            else:
                kw["bias"] = bias
        if scale is not None:
            if isinstance(scale, V):
                kw["scale"] = scale.ap
                rd.append(scale.b)
            else:
                kw["scale"] = scale
        if accum is not None:
            kw["accum_out"] = accum.ap
            wr.append(accum.b)
        P.op("act", lambda e: e.activation(out=out.ap, in_=in_.ap, func=func, **kw), reads=rd, writes=wr)

    def ts(out, in0, s1, s2, op0, op1=None, eng="dve"):
        rd = [in0.b]
        a1 = s1
        a2 = s2
        if isinstance(s1, V):
            a1 = s1.ap
            rd.append(s1.b)
        if isinstance(s2, V):
            a2 = s2.ap
            rd.append(s2.b)
        if op1 is None:
            P.op(eng, lambda e: e.tensor_scalar(out=out.ap, in0=in0.ap, scalar1=a1, scalar2=None, op0=op0),
                 reads=rd, writes=[out.b])
        else:
            P.op(eng, lambda e: e.tensor_scalar(out=out.ap, in0=in0.ap, scalar1=a1, scalar2=a2, op0=op0, op1=op1),
                 reads=rd, writes=[out.b])

    def stt(out, in0, s, in1, op0, op1):
        rd = [in0.b, in1.b]
        a = s
        if isinstance(s, V):
            a = s.ap
            rd.append(s.b)
        P.op("dve", lambda e: e.scalar_tensor_tensor(out=out.ap, in0=in0.ap, scalar=a, in1=in1.ap, op0=op0, op1=op1),
             reads=rd, writes=[out.b])

    def tt(out, in0, in1, op, eng="dve"):
        P.op(eng, lambda e: e.tensor_tensor(out=out.ap, in0=in0.ap, in1=in1.ap, op=op),
             reads=[in0.b, in1.b], writes=[out.b])

    def cp(out, in_, eng="dve"):
        P.op(eng, lambda e: e.tensor_copy(out=out.ap, in_=in_.ap), reads=[in_.b], writes=[out.b])

    def red(out, in_, op, negate=False):
        P.op("dve", lambda e: e.tensor_reduce(out=out.ap, in_=in_.ap, axis=AX.X, op=op, negate=negate),
             reads=[in_.b], writes=[out.b])

    def memset(out, val, eng="dve"):
        P.op(eng, lambda e: e.memset(out.ap, val), writes=[out.b])

    def ld(out, in_, q="sp"):
        P.dma(q, lambda e: e.dma_start(out=out.ap, in_=in_.ap), reads=[in_.b], writes=[out.b])

    def ldc(out, in_):
        P.dma("pool", lambda e: e.dma_start(out=out.ap, in_=in_.ap), reads=[in_.b], writes=[out.b])

    def ldb(out, in_, n):
        P.dma("sp", lambda e: e.dma_start(out=out.ap, in_=in_.ap.to_broadcast([128, n])), reads=[in_.b], writes=[out.b])

    grp4 = [[0, 1, 2, 3], [4, 5, 6, 7]]
    pairs = [[0, 4], [1, 5], [2, 6], [3, 7]]
    cc_n = [0]

    def ag8(src, dst):
        rows, cols = src.ap.shape
        cc_n[0] += 1
        mid = V(nc.dram_tensor("ccmid%d" % cc_n[0], [4 * rows, cols], src.ap.dtype).ap(), Buf("ccmid"))
        P.collective(lambda e: e.collective_compute("AllGather", ALU.bypass, replica_groups=grp4,
                                                    ins=[src.ap], outs=[mid.ap]), reads=[src.b], writes=[mid.b])
        P.collective(lambda e: e.collective_compute("AllGather", ALU.bypass, replica_groups=pairs,
                                                    ins=[mid.ap], outs=[dst.ap]), reads=[mid.b], writes=[dst.b])

    def rs8(src, dst):
        rows, cols = src.ap.shape
        cc_n[0] += 1
        mid = V(nc.dram_tensor("ccmid%d" % cc_n[0], [rows // 2, cols], src.ap.dtype).ap(), Buf("ccmid"))
        P.collective(lambda e: e.collective_compute("ReduceScatter", ALU.add, replica_groups=pairs,
                                                    ins=[src.ap], outs=[mid.ap]), reads=[src.b], writes=[mid.b])
        P.collective(lambda e: e.collective_compute("ReduceScatter", ALU.add, replica_groups=grp4,
                                                    ins=[mid.ap], outs=[dst.ap]), reads=[mid.b], writes=[dst.b])


    ckpt('p0')
    def const_tile(name, shape, dt):
        return V(P.es.enter_context(nc.sbuf_tensor(name, list(shape), dt))[:], Buf(name))

    ident = const_tile("ident_sb", [128, 128], F32)
    ld(ident, ident_d)
    identb = const_tile("identb_sb", [128, 128], BF16)
    cp(identb, ident)
    gsig = const_tile("gsig", [128, NB, H * 3], F32)
    ones_f = const_tile("ones_f", [128, 128], F32)
    memset(ones_f, 1.0)
    eps_t = const_tile("eps_t", [128, 1], F32)
    memset(eps_t, 1e-5)

    w_in_v = w_in_f.re("(k p) n -> p k n", p=128)

    new_phase()
    xT_v = xT_d.re("(k p) n -> p k n", p=128)
    xg = [A16.alloc([DK, BPG * BW], "xg%d" % i) for i in range(2)]
    wt = [A16.alloc([DK, 512], "wt%d" % i) for i in range(2)]
    stg = [A16.alloc([512], "stg%d" % i) for i in range(2)]
    stg32 = A32.alloc([512], "stg32")
    wcount = [0]

    def load_w_cols(c0, n):
        t = wt[wcount[0] % 2]
        wcount[0] += 1
        ldc(t[:, :, 0:n], w_in_v[:, :, c0:c0 + n])
        return t

    kT_send_v = kT_send.re("(a p) n -> p a n", p=128)
    qT_v = qT_s.re("(a p) n -> p a n", p=128)
    ktypes = ("kc", "vc", "ks", "kw")
    for tg in range(NTG):
        x = xg[tg % 2]
        ldc(x, xT_v[:, :, tg * BPG * BW:(tg + 1) * BPG * BW])
        x3 = x.re("p k (b w) -> p k b w", w=BW)
        fm_cols = [(off["q"] + h * 128, ("q", h)) for h in range(H)]
        for ti, nm in enumerate(ktypes):
            for g in range(G):
                fm_cols.append((off[nm] + g * 128, ("k", ti * G + g)))
        for ci in range(0, len(fm_cols), 4):
            chunk = fm_cols[ci:ci + 4]
            contiguous = all(chunk[j][0] == chunk[0][0] + 128 * j for j in range(len(chunk)))
            for j, (c0, dest) in enumerate(chunk):
                if contiguous:
                    if j == 0:
                        w = load_w_cols(chunk[0][0], 128 * len(chunk))
                    wsl = w[:, :, j * 128:(j + 1) * 128]
                else:
                    w = load_w_cols(c0, 128)
                    wsl = w[:, :, 0:128]
                pb = banks[(ci + j) % 2]
                po = pb[:, 0:TG].re("p (b w) -> p b w", w=128)
                for k in range(DK):
                    mm(po, wsl[:, k, :], x3[:, k, :, HALO:BW], k == 0, k == DK - 1)
                st = stg[(ci + j) % 2]
                cp(st[:, 0:TG], pb[:, 0:TG], eng="dve" if j % 2 == 0 else "dve")
                if dest[0] == "q":
                    ld(qT_v[:, dest[1], tg * TG:(tg + 1) * TG], st[:, 0:TG])
                else:
                    ld(kT_send_v[:, dest[1], tg * TG:(tg + 1) * TG], st[:, 0:TG])
        wv = []
        for nm in ("vs", "vw"):
            wv.append(load_w_cols(off[nm], G * 128))
            for b in range(BPG):
                pb = banks[2 + b % 2]
                for k in range(DK):
                    mm(pb[:, 0:G * 128], x3[:, k, b, HALO:BW], wv[-1][:, k, 0:G * 128], k == 0, k == DK - 1)
                st = stg[b % 2]
                cp(st[:, 0:G * 128], pb[:, 0:G * 128])
                tsel = 0 if nm == "vs" else 1
                row0 = (tg * BPG + b) * 128
                ld(v_send[row0:row0 + 128, tsel * G * 128:(tsel + 1) * G * 128], st[:, 0:G * 128])
        wgn = load_w_cols(off["gn"], H * 3)
        for b in range(BPG):
            pb = banks[2 + b % 2]
            for k in range(DK):
                mm(pb[:, 0:H * 3], x3[:, k, b, HALO:BW], wgn[:, k, 0:H * 3], k == 0, k == DK - 1)
            act(gsig[:, tg * BPG + b, :], pb[:, 0:H * 3], AF.Sigmoid)

    ckpt('pA0')
    for a_ in range(4 * G):
        P.collective(lambda e, a_=a_: e.collective_compute("AllGather", ALU.bypass, replica_groups=grp4,
                                                           ins=[kT_send.ap[a_ * 128:(a_ + 1) * 128, :]],
                                                           outs=[kT_all[a_].ap]),
                     reads=[kT_send.b], writes=[kT_all[a_].b])
    for a_ in range(NVC):
        P.collective(lambda e, a_=a_: e.collective_compute("AllGather", ALU.bypass, replica_groups=grp4,
                                                           ins=[v_send.ap[a_ * NBV * 128:(a_ + 1) * NBV * 128, :]],
                                                           outs=[v_all[a_].ap]),
                     reads=[v_send.b], writes=[v_all[a_].b])

    ckpt('pA')
    kT_all_v = [t_.re("(r p) n -> p r n", p=128) for t_ in kT_all]
    v_all_v = [t_.re("(r i p) c -> p r i c", p=128, i=NBV) for t_ in v_all]
    onT_v = onT_s.re("(a p) n -> p a n", p=128)
    NCH = (NCMP + 127) // 128
    HB = min(4, HPG)
    KcT = const_tile("KcT", [128, 512], BF16)
    Vc = const_tile("Vc", [128, NCH, 130], BF16)

    for g in range(G):
        new_phase()
        Xc = A16.alloc([S], "Xc")
        w1s = A16.alloc([32, 128], "w1s")
        w2s = A16.alloc([128], "w2s")
        peT = A16.alloc([32], "peT")
        hid = A16.alloc([512], "hid")
        b1s = A32.alloc([1], "b1s")
        b2s = A32.alloc([1], "b2s")
        b2r = A32.alloc([128], "b2r")
        pbias = A32.alloc([1], "pbias")
        gx = A32.alloc([512], "gx")
        gy = A32.alloc([512], "gy")
        ckpt('c00')
        cp(Vc[:, :, 128:130], ones_f[:, 0:2 * NCH].re("p (a b) -> p a b", b=2))
        ckpt('c0')
        for t in range(2):
            d4 = Xc.re("p (i r q) -> p i r q", r=CPB, q=128)
            for r in range(CPB):
                ld(d4[:, :, r, :], kT_all_v[t * G + g][:, r, :].re("p (i q) -> p i q", q=128))
            ckpt('c0a')
            ldc(w1s, w1_d[t].re("(l p) o -> p l o", p=128))
            ldc(w2s, w2_d[t])
            ldc(peT, peT_d[t])
            ckpt('c0b')
            ld(b1s, b1_d[t])
            ld(b2s, b2_d[t])
            ldb(b2r, b2r_d[t], 128)
            ckpt('c1')
            ph = banks[0]
            pp = banks[1]
            for l in range(32):
                mm(ph[:, 0:NCMP], w1s[:, l, :], Xc[:, l:l + 16 * (NCMP - 1) + 1:16], l == 0, l == 31)
            ckpt('c2')
            for l in range(32):
                mm(pp[:, 0:1], w1s[:, l, :], peT[:, l:l + 1], l == 0, l == 31)
            ckpt('c3')
            tt(pbias, pp[:, 0:1], b1s, ALU.add)
            act(gx[:, 0:NCMP], ph[:, 0:NCMP], AF.Identity, bias=pbias)
            tt(gy[:, 0:NCMP], gx[:, 0:NCMP], gx[:, 0:NCMP], ALU.mult)
            ts(gy[:, 0:NCMP], gy[:, 0:NCMP], 0.044715, 1.0, ALU.mult, ALU.add)
            tt(gy[:, 0:NCMP], gy[:, 0:NCMP], gx[:, 0:NCMP], ALU.mult)
            act(gy[:, 0:NCMP], gy[:, 0:NCMP], AF.Tanh, scale=0.7978845608028654)
            stt(gy[:, 0:NCMP], gy[:, 0:NCMP], 1.0, gx[:, 0:NCMP], ALU.add, ALU.mult)
            ts(hid[:, 0:NCMP], gy[:, 0:NCMP], 0.5, None, ALU.mult)
            ckpt('c4')
            if t == 0:
                mm(pp[:, 0:NCMP], w2s, hid[:, 0:NCMP], True, True)
                act(KcT[:, 0:NCMP], pp[:, 0:NCMP], AF.Identity, bias=b2s)
            else:
                for ch in range(NCH):
                    n0 = ch * 128
                    nn = min(128, NCMP - n0)
                    mm(pp[0:nn, 0:128], hid[:, n0:n0 + nn], w2s, True, True)
                    tt(Vc[0:nn, ch, 0:128], pp[0:nn, 0:128], b2r[0:nn, :], ALU.add)
        ckpt('pCc')
        new_phase()
        KsT = A16.alloc([S], "KsT")
        KwT = A16.alloc([S], "KwT")
        Vs = A16.alloc([NQ, 130], "Vs")
        Vw = A16.alloc([NQ, 130], "Vw")
        QTb = [A16.alloc([HPG, 128], "QT%d" % i_) for i_ in range(2)]
        wmask = A16.alloc([8, 128], "wmask")
        camask = A16.alloc([4, 128], "camask")
        alibi = A32.alloc([H, NQ + 3], "alibi")
        cmpb = A32.alloc([HPG, MC], "cmpb")
        selF = A32.alloc([8 * NB], "selF")
        selV = A32.alloc([8 * NB], "selV")
        ld(wmask, wmask_d.re("p (a b) -> p a b", b=128))
        ld(camask, camask_d.re("p (a b) -> p a b", b=128))
        ld(alibi, alibi_d.re("p (a b) -> p a b", b=NQ + 3))
        ld(cmpb, cmpb_d.re("p (a b) -> p a b", b=MC)[:, g * HPG:(g + 1) * HPG, :])
        ld(selF, selF_d)
        ld(selV, selV_d)
        for (dst, ti) in ((KsT, 2), (KwT, 3)):
            d4 = dst.re("p (i r q) -> p i r q", r=CPB, q=128)
            for r in range(CPB):
                ld(d4[:, :, r, :], kT_all_v[ti * G + g][:, r, :].re("p (i q) -> p i q", q=128))
        for (dst, tsel) in ((Vs, 0), (Vw, 1)):
            cp(dst[:, :, 128:130], ones_f[:, 0:2 * NQ].re("p (a b) -> p a b", b=2))
            d4 = dst.re("p (i r) c -> p i r c", r=CPB)
            for r in range(CPB):
                for a_ in range(NVC):
                    ld(d4[:, a_ * NBV:(a_ + 1) * NBV, r, 0:128],
                       v_all_v[a_][:, r, :, (tsel * G + g) * 128:(tsel * G + g + 1) * 128])
        sc = A32.alloc([512], "sc")
        ex = A32.alloc([512], "ex")
        imp = A32.alloc([4 * 8 * NB + 8], "imp")
        isel = A32.alloc([8 * NB], "isel")
        score = A32.alloc([8 * NB], "score")
        scr2 = A32.alloc([8 * NB], "scr2")
        mx8 = A32.alloc([8], "mx8")
        msk = A32.alloc([8 * NB], "msk")
        mE = [A32.alloc([2, 64], "mE%d" % i_) for i_ in range(2)]
        MT = A16.alloc([NQ, 128], "MT")
        eT = A16.alloc([NCH, 128], "eT")
        pT = [A16.alloc([HB, 128], "pT%d" % i) for i in range(2)]
        small = A32.alloc([16], "small")
        ocmp = A32.alloc([HPG, 129], "ocmp")
        oacc = A32.alloc([128], "oacc")
        wcol = A32.alloc([8], "wcol")
        onT_st = A16.alloc([HPG, 128], "onT_st")
        for i in range(NB):
            Ni = min(32 * i + 31, NCMP)
            Ji = min(8 * i + 8, NSEL)
            m0 = 32 * (NB - 1 - i)
            f0 = 8 * (NB - 1 - i)
            nch_i = (Ni + 127) // 128
            memset(imp, 0.0)
            QT = QTb[i % 2]
            ld(QT, qT_v[:, g * HPG:(g + 1) * HPG, i * 128:(i + 1) * 128])
            ckpt('a0')
            for h in range(HPG):
                ps = banks[h % 2]
                mm(ps[:, 0:Ni], QT[:, h, :], KcT[:, 0:Ni], True, True)
                stt(sc[:, 0:Ni], ps[:, 0:Ni], scale, cmpb[:, h, m0:m0 + Ni], ALU.mult, ALU.add)
                red(small[:, 0:1], sc[:, 0:Ni], ALU.max)
                ts(small[:, 1:2], small[:, 0:1], -1.0, 20000.0, ALU.mult, ALU.min)
                act(ex[:, 0:Ni], sc[:, 0:Ni], AF.Exp, bias=small[:, 1:2])
                red(small[:, 2:3], ex[:, 0:Ni], ALU.add)
                ts(small[:, 3:4], small[:, 2:3], TINY, None, ALU.max)
                P.op("dve", lambda e, small=small: e.reciprocal(out=small.ap[:, 4:5], in_=small.ap[:, 3:4]),
                     reads=[small.b], writes=[small.b])
                stt(imp[:, 1:1 + Ni], ex[:, 0:Ni], small[:, 4:5], imp[:, 1:1 + Ni], ALU.mult, ALU.add)
                ckpt('a0c')
                for ch in range(nch_i):
                    n0 = ch * 128
                    nn = min(128, Ni - n0)
                    pt = banks[2 + ch % 2]
                    tr(pt[0:nn, 0:128], ex[:, n0:n0 + nn], ident)
                    cp(eT[0:nn, ch, :], pt[0:nn, 0:128])
                ckpt('a0d')
                po = banks[4]
                for ch in range(nch_i):
                    n0 = ch * 128
                    nn = min(128, Ni - n0)
                    mm(po[:, 0:129], eT[0:nn, ch, :], Vc[0:nn, ch, 0:129], ch == 0, ch == nch_i - 1)
                cp(ocmp[:, h, :], po[:, 0:129])
            ckpt('a1')
            impv = imp[:, 0:4 * Ji].re("p (j f) -> p j f", f=4)
            red(isel[:, 0:Ji], impv, ALU.add)
            tt(isel[:, 0:Ji], isel[:, 0:Ji], imp[:, 4:4 + 4 * Ji].re("p (j f) -> p j f", f=4)[:, :, 0], ALU.add)
            tt(score[:, 0:Ji], isel[:, 0:Ji], selF[:, f0:f0 + Ji], ALU.add)
            ts(score[:, 0:1], score[:, 0:1], 1.0e4, None, ALU.add)
            if Ji > TOPN:
                cur = score
                for rnd in range(TOPN // 8):
                    P.op("dve", lambda e, cur=cur, Ji=Ji, mx8=mx8: e.max(out=mx8.ap, in_=cur.ap[:, 0:Ji]), reads=[cur.b], writes=[mx8.b])
                    if rnd < TOPN // 8 - 1:
                        P.op("dve", lambda e, cur=cur, Ji=Ji, mx8=mx8, scr2=scr2: e.match_replace(out=scr2.ap[:, 0:Ji], in_to_replace=mx8.ap,
                                                                      in_values=cur.ap[:, 0:Ji], imm_value=-1.0e9),
                             reads=[cur.b, mx8.b], writes=[scr2.b])
                        cur = scr2
                stt(msk[:, 0:Ji], score[:, 0:Ji], mx8[:, 7:8], selV[:, f0:f0 + Ji], ALU.is_ge, ALU.mult)
            else:
                cp(msk[:, 0:Ji], selV[:, f0:f0 + Ji])
            ckpt('a1b')
            ntile = min(4 * i + 4, NQ)
            for j2 in range(ntile):
                pm = banks[2 + j2 % 2]
                me = mE[j2 % 2]
                P.op("dve", lambda e, me=me, j2=j2, msk=msk: e.tensor_copy(
                    out=me.ap, in_=msk.ap[:, 2 * j2:2 * j2 + 2].unsqueeze(2).to_broadcast([128, 2, 64])),
                    reads=[msk.b], writes=[me.b])
                tr(pm[:, 0:128], me.re("p a b -> p (a b)"), ident)
                if j2 >= 4 * i:
                    tt(MT[:, j2, :], pm[:, 0:128], camask[:, j2 - 4 * i, :], ALU.mult)
                else:
                    cp(MT[:, j2, :], pm[:, 0:128])
            ckpt('a2')
            for hb in range(HPG // HB):
                for br in range(2):
                    if br == 0:
                        tiles = list(range(ntile))
                        KT, VV = KsT, Vs
                    else:
                        tiles = [j for j in range(4 * i - 4, 4 * i + 4) if 0 <= j < NQ]
                        KT, VV = KwT, Vw
                    pos_ = [banks[4 + hh] for hh in range(HB)]
                    def s_mm(n_):
                        j2 = tiles[n_]
                        mm(banks[n_ % 2][:, 0:HB * 128].re("p (h q) -> p h q", q=128), KT[:, j2 * 128:(j2 + 1) * 128],
                           QT[:, hb * HB:(hb + 1) * HB, :], True, True)

                    s_mm(0)
                    for n_, j2 in enumerate(tiles):
                        if n_ + 1 < len(tiles):
                            s_mm(n_ + 1)
                        pS = banks[n_ % 2]
                        u = 4 * i - j2 + 3
                        pt_ = pT[n_ % 2]
                        for hh in range(HB):
                            hglob = g * HPG + hb * HB + hh
                            act(pt_[:, hh, :], pS[:, hh * 128:(hh + 1) * 128], AF.Exp,
                                bias=alibi[:, hglob, u:u + 1], scale=scale)
                        if br == 0:
                            mk = MT[:, j2, :]
                        else:
                            mk = wmask[:, j2 - (4 * i - 4), :]
                        for hh in range(HB):
                            tt(pt_[:, hh, :], pt_[:, hh, :], mk, ALU.mult, eng="pool" if hh % 2 else "dve")
                        for hh in range(HB):
                            mm(pos_[hh][:, 0:129], pt_[:, hh, :], VV[:, j2, 0:129], n_ == 0, n_ == len(tiles) - 1)
                    for hh in range(HB):
                        h = hb * HB + hh
                        hglob = g * HPG + h
                        po = pos_[hh]
                        ts(wcol[:, 0:1], po[:, 128:129], TINY, None, ALU.max)
                        P.op("dve", lambda e, wcol=wcol: e.reciprocal(out=wcol.ap[:, 1:2], in_=wcol.ap[:, 0:1]),
                             reads=[wcol.b], writes=[wcol.b])
                        tt(wcol[:, 2:3], wcol[:, 1:2], gsig[:, i, hglob * 3 + 1 + br:hglob * 3 + 2 + br], ALU.mult)
                        if br == 0:
                            ts(wcol[:, 3:4], ocmp[:, h, 128:129], TINY, None, ALU.max)
                            P.op("dve", lambda e, wcol=wcol: e.reciprocal(out=wcol.ap[:, 4:5], in_=wcol.ap[:, 3:4]),
                                 reads=[wcol.b], writes=[wcol.b])
                            tt(wcol[:, 5:6], wcol[:, 4:5], gsig[:, i, hglob * 3:hglob * 3 + 1], ALU.mult)
                            ts(ocmp[:, h, 0:128], ocmp[:, h, 0:128], wcol[:, 5:6], None, ALU.mult)
                            stt(ocmp[:, h, 0:128], po[:, 0:128], wcol[:, 2:3], ocmp[:, h, 0:128], ALU.mult, ALU.add)
                        else:
                            stt(oacc, po[:, 0:128], wcol[:, 2:3], ocmp[:, h, 0:128], ALU.mult, ALU.add)
                            ptr = banks[2 + hh % 2]
                            tr(ptr[:, 0:128], oacc, ident)
                            cp(onT_st[:, h, :], ptr[:, 0:128])
            ckpt('a3')
            ld(onT_v[:, g * HPG:(g + 1) * HPG, i * 128:(i + 1) * 128], onT_st)

    ckpt('pC')
    w_nsa_v = w_nsa_f.re("(k p) n -> p k n", p=128)
    w_cp_v = w_cp_f.re("(k p) n -> p k n", p=128)
    w_out_v = w_out_f.re("(k p) n -> p k n", p=128)
    x1T_send_v = x1T_send.re("(k p) n -> p k n", p=128)
    convw_v = convw_d.re("(k p) t -> p k t", p=128)
    for tg in range(NTG):
        new_phase()
        NW = BPG * BW
        x = A16.alloc([DK, NW], "xgD")
        ldc(x, xT_v[:, :, tg * NW:(tg + 1) * NW])
        x3 = x.re("p k (b w) -> p k b w", w=BW)
        wtD = [A16.alloc([DK, 128], "wtD%d" % i_) for i_ in range(2)]
        mT = A16.alloc([DK, TG], "mT")
        mark = AR.pos
        convw = A32.alloc([CK, 31], "convw")
        convb = A32.alloc([CK], "convb")
        clng = A32.alloc([CK], "clng")
        clnb = A32.alloc([CK], "clnb")
        ld(convw, convw_v)
        ld(convb, convb_d)
        ld(clng, clng_d)
        ld(clnb, clnb_d)
        hglu = A32.alloc([NW], "hglu")
        sg = A32.alloc([NW], "sg")
        hc = A32.alloc([CK, TG], "hc")
        hsq = A32.alloc([TG], "hsq")
        mean = A32.alloc([TG], "mean")
        rstd = A32.alloc([TG], "rstd")
        sT = A16.alloc([CK, TG], "sT")
        p_sum = banks[6]
        p_sq = banks[7]
        for ck in range(CK):
            wa = wtD[0]
            wb = wtD[1]
            ldc(wa[:, :, 0:128], w_in_v[:, :, off["glu"] + ck * 128:off["glu"] + (ck + 1) * 128])
            ldc(wb[:, :, 0:128], w_in_v[:, :, off["glu"] + CH + ck * 128:off["glu"] + CH + (ck + 1) * 128])
            for b0 in range(0, BPG, 3):
                nb_ = min(3, BPG - b0)
                pa = banks[0]
                pbk = banks[1]
                oa = pa[:, 0:nb_ * BW].re("p (b w) -> p b w", w=BW)
                ob = pbk[:, 0:nb_ * BW].re("p (b w) -> p b w", w=BW)
                for k in range(DK):
                    mm(oa, wa[:, k, 0:128], x3[:, k, b0:b0 + nb_, :], k == 0, k == DK - 1)
                for k in range(DK):
                    mm(ob, wb[:, k, 0:128], x3[:, k, b0:b0 + nb_, :], k == 0, k == DK - 1)
                act(sg[:, b0 * BW:(b0 + nb_) * BW], pbk[:, 0:nb_ * BW], AF.Sigmoid)
                tt(hglu[:, b0 * BW:(b0 + nb_) * BW], pa[:, 0:nb_ * BW], sg[:, b0 * BW:(b0 + nb_) * BW], ALU.mult)
            h3 = hglu.re("p (b w) -> p b w", w=BW)
            hcv = hc[:, ck, :].re("p (b q) -> p b q", q=128)
            ts(hcv, h3[:, :, 0:128], convw[:, ck, 0:1], convb[:, ck:ck + 1], ALU.mult, ALU.add)
            for tap in range(1, 31):
                stt(hcv, h3[:, :, tap:tap + 128], convw[:, ck, tap:tap + 1], hcv, ALU.mult, ALU.add)
            act(hsq, hc[:, ck, :], AF.Square)
            mm(p_sum[:, 0:TG], ones_f, hc[:, ck, :], ck == 0, ck == CK - 1)
            mm(p_sq[:, 0:TG], ones_f, hsq, ck == 0, ck == CK - 1)
        ckpt('d1')
        ts(mean, p_sum[:, 0:TG], 1.0 / CH, None, ALU.mult)
        tt(rstd, mean, mean, ALU.mult)
        stt(rstd, p_sq[:, 0:TG], 1.0 / CH, rstd, ALU.mult, ALU.subtract)
        ts(rstd, rstd, 0.0, None, ALU.max)
        act(rstd, rstd, AF.Sqrt, bias=eps_t)
        P.op("dve", lambda e, rstd=rstd: e.reciprocal(out=rstd.ap, in_=rstd.ap), reads=[rstd.b], writes=[rstd.b])
        for ck in range(CK):
            tt(hc[:, ck, :], hc[:, ck, :], mean, ALU.subtract)
            tt(hc[:, ck, :], hc[:, ck, :], rstd, ALU.mult)
            act(sT[:, ck, :], hc[:, ck, :], AF.Silu, bias=clnb[:, ck:ck + 1], scale=clng[:, ck:ck + 1])
        ckpt('d2')
        onT = A16.alloc([H, TG], "onT")
        ld(onT, onT_v[:, :, tg * TG:(tg + 1) * TG])
        wn = [A16.alloc([H, 128], "wn%d" % i_) for i_ in range(2)]
        wc = [A16.alloc([CK, 128], "wc%d" % i_) for i_ in range(2)]
        ga = A32.alloc([TG], "ga")
        gb = A32.alloc([TG], "gb")
        for dm in range(DK):
            wa = wtD[0]
            wb = wtD[1]
            ldc(wa[:, :, 0:128], w_in_v[:, :, off["mrg"] + dm * 128:off["mrg"] + (dm + 1) * 128])
            ldc(wb[:, :, 0:128], w_in_v[:, :, off["mrg"] + D + dm * 128:off["mrg"] + D + (dm + 1) * 128])
            wnn = wn[dm % 2]
            wcc = wc[dm % 2]
            ldc(wnn, w_nsa_v[:, :, dm * 128:(dm + 1) * 128])
            ldc(wcc, w_cp_v[:, :, dm * 128:(dm + 1) * 128])
            pga, pgb, pya, pyb = banks[0], banks[1], banks[2], banks[3]
            oga = pga[:, 0:TG].re("p (b q) -> p b q", q=128)
            ogb = pgb[:, 0:TG].re("p (b q) -> p b q", q=128)
            for k in range(DK):
                mm(oga, wa[:, k, 0:128], x3[:, k, :, HALO:BW], k == 0, k == DK - 1)
            for k in range(DK):
                mm(ogb, wb[:, k, 0:128], x3[:, k, :, HALO:BW], k == 0, k == DK - 1)
            for k in range(H):
                mm(pya[:, 0:TG], wnn[:, k, :], onT[:, k, :], k == 0, k == H - 1)
            for k in range(CK):
                mm(pyb[:, 0:TG], wcc[:, k, :], sT[:, k, :], k == 0, k == CK - 1)
            act(ga, pga[:, 0:TG], AF.Sigmoid)
            act(gb, pgb[:, 0:TG], AF.Sigmoid)
            tt(ga, ga, pya[:, 0:TG], ALU.mult)
            tt(gb, gb, pyb[:, 0:TG], ALU.mult)
            tt(mT[:, dm, :], ga, gb, ALU.add)
        ckpt('d3')
        P.barrier()
        AR.pos = mark
        wo = [A16.alloc([DK, 512], "wo%d" % i_) for i_ in range(2)]
        xin = A32.alloc([D], "xin")
        stats = A32.alloc([8, 6], "stats")
        mv = A32.alloc([4], "mv")
        lg = A32.alloc([D], "lg")
        lb = A32.alloc([D], "lb")
        ldb(lg, ln1g_d, D)
        ldb(lb, ln1b_d, D)
        NDC = (D + 511) // 512
        x1b = A32.alloc([D], "x1b")
        xTst = A16.alloc([DK, 128], "xTst")
        rws = A32.alloc([DK, E], "rws")
        ld(rws, rw_d.re("(k p) e -> p k e", p=128))
        rbs = A32.alloc([E], "rbs")
        ldb(rbs, rb_d, E)
        x1T32 = A32.alloc([DK, 128], "x1T32")
        lgt = A32.alloc([E], "lgt")
        lg2 = A32.alloc([E], "lg2")
        gts = A32.alloc([E], "gts")
        mxr = A32.alloc([8], "mxr")
        sm = A32.alloc([4], "sm")
        for b in range(BPG):
            row0 = (tg * BPG + b) * 128
            ld(xin, xtok_d[row0:row0 + 128, :])
            for dc in range(NDC):
                cw = min(512, D - dc * 512)
                w = wo[dc % 2]
                if b == 0 or NDC > 2:
                    ldc(w[:, :, 0:cw], w_out_v[:, :, dc * 512:dc * 512 + cw])
                pm = banks[4 + dc % 2]
                for k in range(DK):
                    mm(pm[:, 0:cw], mT[:, k, b * 128:(b + 1) * 128], w[:, k, 0:cw], k == 0, k == DK - 1)
                stt(xin[:, dc * 512:dc * 512 + cw], xin[:, dc * 512:dc * 512 + cw], ALPHA, pm[:, 0:cw], ALU.mult, ALU.add)
                P.op("dve", lambda e, dc=dc, cw=cw, stats=stats, xin=xin: e.bn_stats(out=stats.ap[:, dc, :], in_=xin.ap[:, dc * 512:dc * 512 + cw]),
                     reads=[xin.b], writes=[stats.b])
            P.op("dve", lambda e, mv=mv, stats=stats, NDC=NDC: e.bn_aggr(out=mv.ap[:, 0:2], in_=stats.ap.rearrange("p a b -> p (a b)")[:, 0:NDC * 6]), reads=[stats.b], writes=[mv.b])
            act(mv[:, 2:3], mv[:, 1:2], AF.Sqrt, bias=eps_t)
            P.op("dve", lambda e, mv=mv: e.reciprocal(out=mv.ap[:, 3:4], in_=mv.ap[:, 2:3]), reads=[mv.b], writes=[mv.b])
            ts(x1b, xin, mv[:, 0:1], mv[:, 3:4], ALU.subtract, ALU.mult)
            tt(x1b, x1b, lg, ALU.mult)
            tt(x1b, x1b, lb, ALU.add)
            ckpt('d4')
            ld(x1_s[row0:row0 + 128, :], x1b)
            for k in range(DK):
                ptr = banks[k % 2]
                tr(ptr[:, 0:128], x1b[:, k * 128:(k + 1) * 128], ident)
                cp(x1T32[:, k, :], ptr[:, 0:128])
                act(xTst[:, k, :], x1T32[:, k, :], AF.Copy)
            ld(x1T_send_v[:, :, row0:row0 + 128], xTst)
            ckpt('d5')
            pr = banks[2]
            for k in range(DK):
                mm(pr[:, 0:E], x1T32[:, k, :], rws[:, k, :], k == 0, k == DK - 1)
            tt(lgt, pr[:, 0:E], rbs, ALU.add)
            P.op("dve", lambda e, mxr=mxr, lgt=lgt: e.max(out=mxr.ap, in_=lgt.ap), reads=[lgt.b], writes=[mxr.b])
            ts(lg2, lgt, mxr[:, 3:4], None, ALU.is_ge)
            ts(gts, lgt, mxr[:, 0:1], None, ALU.subtract)
            act(gts, gts, AF.Exp)
            tt(gts, gts, lg2, ALU.mult)
            red(sm[:, 0:1], gts, ALU.add)
            P.op("dve", lambda e, sm=sm: e.reciprocal(out=sm.ap[:, 1:2], in_=sm.ap[:, 0:1]), reads=[sm.b], writes=[sm.b])
            ts(gts, gts, sm[:, 1:2], None, ALU.mult)
            ld(gate_send[row0:row0 + 128, :], gts)

    ckpt('pD')
    for a_ in range(DK):
        ag8(x1T_send[a_ * 128:(a_ + 1) * 128, :], x1T_all[a_])
    ag8(gate_send, gate_all)
    x1T_all_v = [t_.re("(r p) n -> p r n", p=128) for t_ in x1T_all]
    hT_v = hT_s.re("(k p) n -> p k n", p=128)
    TT = min(512, NT)
    NTT = NT // TT
    FH = max(1, FK // 8)
    FC = FK // FH
    for el in range(EPC):
        for fh in range(FH):
            new_phase()
            wgs = A16.alloc([DK, FC * 128], "wgs")
            wus = A16.alloc([DK, FC * 128], "wus")
            ldc(wgs, wg_d[el].re("(k p) f -> p k f", p=128)[:, :, fh * FC * 128:(fh + 1) * FC * 128])
            ldc(wus, wu_d[el].re("(k p) f -> p k f", p=128)[:, :, fh * FC * 128:(fh + 1) * FC * 128])
            bgs = A32.alloc([FC], "bgs")
            bus = A32.alloc([FC], "bus")
            ld(bgs, bg_d[el][:, fh * FC:(fh + 1) * FC])
            ld(bus, bu_d[el][:, fh * FC:(fh + 1) * FC])
            xt = [A16.alloc([DK, TT], "xt%d" % i_) for i_ in range(2)]
            hst = [A16.alloc([TT], "hst%d" % i_) for i_ in range(2)]
            gg = A32.alloc([TT], "gg")
            uu = A32.alloc([TT], "uu")
            sgm = A32.alloc([TT], "sgm")
            xtiles = [(r, t) for r in range(NCORES) for t in range(NTT)]

            def load_x(n_):
                r, t = xtiles[n_]
                for k in range(DK):
                    ld(xt[n_ % 2][:, k, :], x1T_all_v[k][:, r, t * TT:(t + 1) * TT])

            load_x(0)
            for n_, (r, t) in enumerate(xtiles):
                if True:
                    if n_ + 1 < len(xtiles):
                        load_x(n_ + 1)
                    xx = xt[n_ % 2]
                    col0 = r * NT + t * TT
                    for fo in range(FC):
                        pg = banks[(2 * fo) % 4]
                        pu = banks[(2 * fo + 1) % 4]
                        for k in range(DK):
                            mm(pg[:, 0:TT], wgs[:, k, fo * 128:(fo + 1) * 128], xx[:, k, :], k == 0, k == DK - 1)
                        for k in range(DK):
                            mm(pu[:, 0:TT], wus[:, k, fo * 128:(fo + 1) * 128], xx[:, k, :], k == 0, k == DK - 1)
                        ts(gg, pg[:, 0:TT], bgs[:, fo:fo + 1], 7.0, ALU.add, ALU.min)
                        ts(uu, pu[:, 0:TT], bus[:, fo:fo + 1], 7.0, ALU.add, ALU.min)
                        ts(uu, uu, -7.0, 1.0, ALU.max, ALU.add)
                        act(sgm, gg, AF.Sigmoid, scale=1.702)
                        tt(gg, gg, sgm, ALU.mult, eng="pool")
                        hs = hst[fo % 2]
                        tt(hs, gg, uu, ALU.mult)
                        ld(hT_v[:, fh * FC + fo, col0:col0 + TT], hs)
        new_phase()
        wds = A16.alloc([FK, D], "wds")
        ldc(wds, wd_d[el].re("(k p) n -> p k n", p=128))
        bdr = A32.alloc([D], "bdr")
        ldb(bdr, bd_d[el], D)
        selm = A32.alloc([EPC, E], "selm")
        ldb(selm.re("p a b -> p (a b)"), selm_d, EPC * E)
        ht = [A16.alloc([FK, 128], "ht%d" % i_) for i_ in range(2)]
        gcol = [A32.alloc([E], "gcol%d" % i_) for i_ in range(2)]
        gtmp = A32.alloc([E], "gtmp")
        gsel = A32.alloc([2], "gsel")
        yo = [A32.alloc([D], "yo%d" % i_) for i_ in range(2)]
        yp = [A32.alloc([D], "yp%d" % i_) for i_ in range(2)]
        NDC2 = (D + 511) // 512
        def load_b(tc):
            ld(ht[tc % 2], hT_v[:, :, tc * 128:(tc + 1) * 128])
            ld(gcol[tc % 2], gate_all[tc * 128:(tc + 1) * 128, :])
            if el > 0:
                ld(yp[tc % 2], part_s[tc * 128:(tc + 1) * 128, :])

        load_b(0)
        for tc in range(TALL // 128):
            if tc + 1 < TALL // 128:
                load_b(tc + 1)
            hh_ = ht[tc % 2]
            gc = gcol[tc % 2]
            tt(gtmp, gc, selm[:, el, :], ALU.mult)
            red(gsel[:, 0:1], gtmp, ALU.add)
            y = yo[tc % 2]
            if el > 0:
                ypv = yp[tc % 2]
            for dc in range(NDC2):
                cw = min(512, D - dc * 512)
                pm = banks[dc % 4]
                for k in range(FK):
                    mm(pm[:, 0:cw], hh_[:, k, :], wds[:, k, dc * 512:dc * 512 + cw], k == 0, k == FK - 1)
                tt(y[:, dc * 512:dc * 512 + cw], pm[:, 0:cw], bdr[:, dc * 512:dc * 512 + cw], ALU.add)
            if el > 0:
                stt(y, y, gsel[:, 0:1], ypv, ALU.mult, ALU.add)
            else:
                ts(y, y, gsel[:, 0:1], None, ALU.mult)
            ld(part_s[tc * 128:(tc + 1) * 128, :], y)

    ckpt('pE')
    rs8(part_s, f_s)

    new_phase()
    lg = A32.alloc([D], "lg2g")
    lb = A32.alloc([D], "lg2b")
    ldb(lg, ln2g_d, D)
    ldb(lb, ln2b_d, D)
    xa = [A32.alloc([D], "xa%d" % i_) for i_ in range(2)]
    fa = [A32.alloc([D], "fa%d" % i_) for i_ in range(2)]
    stats = A32.alloc([8, 6], "stats2")
    mv = A32.alloc([4], "mv2")
    NDC = (D + 511) // 512
    for b in range(NB):
        xv = xa[b % 2]
        fv = fa[b % 2]
        ld(xv, x1_s[b * 128:(b + 1) * 128, :])
        ld(fv, f_s[b * 128:(b + 1) * 128, :])
        stt(xv, xv, ALPHA, fv, ALU.mult, ALU.add)
        for dc in range(NDC):
            cw = min(512, D - dc * 512)
            P.op("dve", lambda e, dc=dc, cw=cw, xv=xv, stats=stats: e.bn_stats(out=stats.ap[:, dc, :], in_=xv.ap[:, dc * 512:dc * 512 + cw]),
                 reads=[xv.b], writes=[stats.b])
        P.op("dve", lambda e, mv=mv, stats=stats, NDC=NDC: e.bn_aggr(out=mv.ap[:, 0:2], in_=stats.ap.rearrange("p a b -> p (a b)")[:, 0:NDC * 6]), reads=[stats.b], writes=[mv.b])
        act(mv[:, 2:3], mv[:, 1:2], AF.Sqrt, bias=eps_t)
        P.op("dve", lambda e, mv=mv: e.reciprocal(out=mv.ap[:, 3:4], in_=mv.ap[:, 2:3]), reads=[mv.b], writes=[mv.b])
        ts(fv, xv, mv[:, 0:1], mv[:, 3:4], ALU.subtract, ALU.mult)
        tt(fv, fv, lg, ALU.mult)
        tt(fv, fv, lb, ALU.add)
        ld(out_d[b * 128:(b + 1) * 128, :], fv)
    P.barrier()
    P.finish()
    return nc, c


def _tables(c, r):
    NQ, NB, H = c["NQ"], c["NB"], c["H"]
    slopes = np.exp2(-8.0 * np.arange(1, H + 1, dtype=np.float64) / H)
    k = np.arange(128, dtype=np.float64)
    u = np.arange(NQ + 3)
    delta = u - 3 + r
    rel = (-128.0 * delta[None, :] + k[:, None] - 64.0)
    al = slopes[None, :, None] * rel[:, None, :]
    al = np.where(delta[None, None, :] >= 0, al, NEG)
    alibi = al.reshape(128, H * (NQ + 3)).astype(np.float32)
    ki = np.arange(128)[:, None]
    qi = np.arange(128)[None, :]
    wm = np.zeros((128, 8, 128), np.float32)
    for dj in range(8):
        d = r + 4 - dj
        if d == 0:
            wm[:, dj, :] = (ki <= qi)
        elif 1 <= d <= 3:
            wm[:, dj, :] = 1.0
        elif d == 4:
            wm[:, dj, :] = (ki > qi)
    ca = np.zeros((128, 4, 128), np.float32)
    for dj in range(4):
        d = r - dj
        if d > 0:
            ca[:, dj, :] = 1.0
        elif d == 0:
            ca[:, dj, :] = (ki <= qi)
    MC = 32 * NB
    m = np.arange(MC)
    npr = m - 32 * (NB - 1) - 8 * r
    dist = np.arange(128)[:, None] - 16.0 * npr[None, :] - 31.0
    cb = -slopes[None, :, None] * dist[:, None, :]
    cb = np.where(dist[:, None, :] >= 0, cb, NEG)
    cmpb = cb.reshape(128, H * MC).astype(np.float32)
    mm_ = np.arange(8 * NB)
    jp = mm_ - 8 * (NB - 1) - 2 * r
    q = np.arange(128)[:, None]
    curp = (q >= 64).astype(np.int64)
    valid = (64 * jp[None, :] <= q)
    forced = (jp[None, :] == curp) | (jp[None, :] == curp - 1)
    selF = np.where(valid, np.where(forced, 1.0e4, 0.0), -1.0e9).astype(np.float32)
    selV = valid.astype(np.float32)
    return dict(alibi_tab=alibi, wmask=wm.reshape(128, 8 * 128).astype(ml_dtypes.bfloat16),
                camask=ca.reshape(128, 4 * 128).astype(ml_dtypes.bfloat16), cmpb=cmpb, selF=selF, selV=selV)


def make_in_maps(c, inp):
    S, D, H, E, CH = c["S"], c["D"], c["H"], c["E"], c["CH"]
    NB, NT, EPC = c["NB"], c["NT"], c["EPC"]
    f32 = lambda a: np.ascontiguousarray(a, dtype=np.float32)
    x = inp["x"]
    shared = dict(
        cmp_peT=f32(inp["cmp_pe"][0].transpose(0, 2, 1)),
        cmp_w1=f32(inp["cmp_w1"][0]),
        cmp_b1=f32(inp["cmp_b1"][0].reshape(2, 128, 1)),
        cmp_w2=f32(inp["cmp_w2"][0]),
        cmp_b2=f32(inp["cmp_b2"][0].reshape(2, 128, 1)),
        cmp_b2r=f32(inp["cmp_b2"][0].reshape(2, 1, 128)),
        conv_wT=f32(inp["conv_w"][0].T),
        conv_b=f32(inp["conv_b"][0].reshape(CH // 128, 128).T),
        conv_ln_g=f32(inp["conv_ln_g"][0].reshape(CH // 128, 128).T),
        conv_ln_b=f32(inp["conv_ln_b"][0].reshape(CH // 128, 128).T),
        ln1_g=f32(inp["ln1_g"][0].reshape(1, D)), ln1_b=f32(inp["ln1_b"][0].reshape(1, D)),
        ln2_g=f32(inp["ln2_g"][0].reshape(1, D)), ln2_b=f32(inp["ln2_b"][0].reshape(1, D)),
        router_w=f32(inp["router_w"][0]), router_b=f32(inp["router_b"][0].reshape(1, E)),
        ident=np.eye(128, dtype=np.float32),
    )
    maps = []
    for k in range(NCORES):
        b, r = k // CPB, k % CPB
        xT = np.zeros((D, NB * BW), np.float32)
        xtok = np.zeros((NT, D), np.float32)
        for i in range(NB):
            cblk = CPB * i + r
            t0 = 128 * cblk - HALO
            lo = max(t0, 0)
            xT[:, i * BW + (lo - t0):(i + 1) * BW] = x[b, lo:128 * cblk + 128].T
            xtok[i * 128:(i + 1) * 128] = x[b, 128 * cblk:128 * cblk + 128]
        m = dict(shared)
        m.update(_tables(c, r))
        selm = np.zeros((EPC, E), np.float32)
        for el in range(EPC):
            selm[el, k * EPC + el] = 1.0
        sh = lambda w: f32(w[k * (w.shape[0] // 8):(k + 1) * (w.shape[0] // 8)])
        m.update(
            xT=xT, xtok=xtok,
            w_in=f32(inp["w_in"][0]), w_nsa=f32(inp["w_nsa_proj"][0]),
            w_cp=f32(inp["w_conv_proj"][0]), w_out=f32(inp["w_out"][0]),
            w_gate=f32(inp["w_gate"][0][k * EPC:(k + 1) * EPC]),
            w_up=f32(inp["w_up"][0][k * EPC:(k + 1) * EPC]),
            w_down=f32(inp["w_down"][0][k * EPC:(k + 1) * EPC]),
            b_gate=f32(inp["b_gate"][0][k * EPC:(k + 1) * EPC].reshape(EPC, -1, 128).transpose(0, 2, 1)),
            b_up=f32(inp["b_up"][0][k * EPC:(k + 1) * EPC].reshape(EPC, -1, 128).transpose(0, 2, 1)),
            b_down=f32(inp["b_down"][0][k * EPC:(k + 1) * EPC].reshape(EPC, 1, D)),
            selm=selm.reshape(1, EPC * E),
        )
        maps.append(m)
    return maps


def run_cfg(cfg, inp, trace=False, stop_after=None):
    nc, c = build_program(cfg, stop_after)
    maps = make_in_maps(c, inp)
    res = run_bass_kernel_spmd(nc, maps, core_ids=list(range(NCORES)))
    S, D, NB = c["S"], c["D"], c["NB"]
    out = np.zeros((2, S, D), np.float32)
    for k in range(NCORES):
        b, r = k // CPB, k % CPB
        o = res.results[k]["out"]
        for i in range(NB):
            cblk = CPB * i + r
            out[b, 128 * cblk:128 * cblk + 128] = o[i * 128:(i + 1) * 128]
    return out


def kernel(**inputs):
    inp = {k: np.asarray(v) for k, v in inputs.items()}
    return run_cfg(CFG_FULL, inp)
```
